# Optimizing a Trainium2 kernel written in Bass

```python
import math
import jax, jax.numpy as jnp
from jax import lax
import numpy as np

D_MODEL = 1024
BATCH = 2
SEQ = 16384
DEPTH = 4

F32 = jnp.float32
GRID_W = 64
CTX_LEN = 256
HEAD_DIM = 64
NA_HEADS = 4
WIN_H = 8
WIN_W = 16
DIFF_HEADS = 4
DIFF_DK = 32
DIFF_DV = 64
MLA_HEADS = 4
MLA_Q_RANK = 256
MLA_KV_RANK = 128
MLA_NOPE = 64
MLA_ROPE = 32
MLA_V = 64
S5_GROUPS = 16
S5_GROUP_CH = 16
S5_STATE = 64
S5_WIDTH = S5_GROUPS * S5_GROUP_CH
N_BRANCH = 4
BRANCH_W = 256
N_EXPERTS = 64
TOP_K = 6
EXPERT_FF = 256
ROUTED_SCALE = 1.0
EXPERT_BLOCK = 256
Q_BLOCK = 128
ROPE_BASE = 10000.0
EPS = 1e-6
NEG_INF = -1e30

NA_W = NA_HEADS * HEAD_DIM
DIFF_QK_W = DIFF_HEADS * 2 * DIFF_DK
DIFF_V_W = DIFF_HEADS * DIFF_DV
IN_SIZES = (3 * NA_W, DIFF_QK_W, DIFF_QK_W, DIFF_V_W, MLA_Q_RANK, MLA_KV_RANK, MLA_ROPE, S5_WIDTH, N_BRANCH * D_MODEL)
IN_W = sum(IN_SIZES)
IN_OFFSETS = tuple(int(v) for v in np.cumsum(IN_SIZES)[:-1])

kernel_name = "hybrid_natten_diff_mla_s5_moe_dit"


def rmsnorm(x, g):
    xf = x.astype(F32)
    y = xf * lax.rsqrt(jnp.mean(xf * xf, axis=-1, keepdims=True) + EPS)
    return (y * g.astype(F32)).astype(x.dtype)


def modulate(h, shift, scale):
    return h * (1 + scale) + shift


def heads_to_tokens(o):
    b, h, l, d = o.shape
    return o.transpose(0, 2, 1, 3).reshape(b, l, h * d)


def _rotate(x, pos):
    n = x.shape[-1]
    half = n // 2
    freqs = ROPE_BASE ** (-jnp.arange(half, dtype=F32) * 2.0 / n)
    ang = pos.astype(F32)[:, None] * freqs[None, :]
    cos, sin = jnp.cos(ang), jnp.sin(ang)
    xf = x.astype(F32)
    x1, x2 = xf[..., :half], xf[..., half:]
    return jnp.concatenate([x1 * cos - x2 * sin, x1 * sin + x2 * cos], axis=-1).astype(x.dtype)


def rope_2d(x, rows, cols):
    r = x.shape[-1] // 2
    return jnp.concatenate([_rotate(x[..., :r], rows), _rotate(x[..., r:], cols)], axis=-1)


def ctx_attention(q, k, v, scale):
    s = jnp.einsum('bhqd,bhkd->bhqk', q, k).astype(F32) * scale
    p = jax.nn.softmax(s, axis=-1).astype(v.dtype)
    return jnp.einsum('bhqk,bhkd->bhqd', p, v)


def attend_latent(q_lat, q_ctx, k_lat, k_ctx, v_lat, v_ctx, scale):
    b, h, s, _ = q_lat.shape
    nb = s // Q_BLOCK

    def to_blocks(q):
        return q.reshape(b, h, nb, Q_BLOCK, q.shape[-1]).transpose(2, 0, 1, 3, 4)

    def one_block(qs):
        ql, qc = qs
        s_lat = jnp.einsum('bhqd,bhkd->bhqk', ql, k_lat).astype(F32)
        s_ctx = jnp.einsum('bhqd,bhkd->bhqk', qc, k_ctx).astype(F32)
        p = jax.nn.softmax(jnp.concatenate([s_lat, s_ctx], axis=-1) * scale, axis=-1).astype(v_lat.dtype)
        return (jnp.einsum('bhqk,bhkd->bhqd', p[..., :s], v_lat)
                + jnp.einsum('bhqk,bhkd->bhqd', p[..., s:], v_ctx))

    o = lax.map(one_block, (to_blocks(q_lat), to_blocks(q_ctx)))
    return o.transpose(1, 2, 0, 3, 4).reshape(b, h, s, -1)


def neighbourhood_attention(qkv_l, qkv_c, rpb, with_ctx_out):
    b, s, _ = qkv_l.shape
    lc = qkv_c.shape[1]
    n_rows = s // GRID_W
    kh = min(WIN_H, n_rows)
    q, k, v = qkv_l.reshape(b, s, 3, NA_HEADS, HEAD_DIM).transpose(2, 0, 3, 1, 4)
    qc, kc, vc = qkv_c.reshape(b, lc, 3, NA_HEADS, HEAD_DIM).transpose(2, 0, 3, 1, 4)
    scale = HEAD_DIM ** -0.5

    def grid(t):
        return t.reshape(b, NA_HEADS, n_rows, GRID_W, HEAD_DIM)

    qg, kg, vg = grid(q), grid(k), grid(v)
    r = jnp.arange(n_rows)
    ridx = jnp.clip(r - kh // 2, 0, n_rows - kh)[:, None] + jnp.arange(kh)[None, :]
    kw = kg[:, :, ridx].reshape(b, NA_HEADS, n_rows, kh * GRID_W, HEAD_DIM)
    vw = vg[:, :, ridx].reshape(b, NA_HEADS, n_rows, kh * GRID_W, HEAD_DIM)
    col = jnp.arange(GRID_W)
    cstart = jnp.clip(col - WIN_W // 2, 0, GRID_W - WIN_W)
    valid = (col[None, :] >= cstart[:, None]) & (col[None, :] < cstart[:, None] + WIN_W)
    dr = ridx - r[:, None] + (WIN_H - 1)
    dc = jnp.clip(col[None, :] - col[:, None], 1 - WIN_W, WIN_W - 1) + (WIN_W - 1)
    bias = rpb.astype(F32)[:, dr[:, None, :, None], dc[None, :, None, :]]
    bias = jnp.where(valid[:, None, :], bias, NEG_INF).reshape(NA_HEADS, n_rows, GRID_W, kh * GRID_W)
    s_win = jnp.einsum('bhrqd,bhrkd->bhrqk', qg, kw).astype(F32) * scale + bias
    s_ctx = jnp.einsum('bhrqd,bhcd->bhrqc', qg, kc).astype(F32) * scale
    p = jax.nn.softmax(jnp.concatenate([s_win, s_ctx], axis=-1), axis=-1).astype(v.dtype)
    nw = kh * GRID_W
    o = (jnp.einsum('bhrqk,bhrkd->bhrqd', p[..., :nw], vw)
         + jnp.einsum('bhrqc,bhcd->bhrqd', p[..., nw:], vc))
    y_l = heads_to_tokens(o.reshape(b, NA_HEADS, s, HEAD_DIM))
    y_c = heads_to_tokens(ctx_attention(qc, kc, vc, scale)) if with_ctx_out else None
    return y_l, y_c


def diff_attention(q_l, k_l, v_l, q_c, k_c, v_c, lam, g_sub, lam_init, rows, cols, with_ctx_out):
    b = q_l.shape[0]

    def qk_heads(t):
        return t.reshape(b, t.shape[1], DIFF_HEADS * 2, DIFF_DK).transpose(0, 2, 1, 3)

    def v_heads(t):
        vv = t.reshape(b, t.shape[1], DIFF_HEADS, DIFF_DV).transpose(0, 2, 1, 3)
        return jnp.repeat(vv, 2, axis=1)

    q, k, v = qk_heads(q_l), qk_heads(k_l), v_heads(v_l)
    kc, vc = qk_heads(k_c), v_heads(v_c)
    scale = DIFF_DK ** -0.5
    lf = lam.astype(F32)
    lam_full = jnp.exp(jnp.sum(lf[0] * lf[1])) - jnp.exp(jnp.sum(lf[2] * lf[3])) + lam_init

    def combine(o):
        l = o.shape[2]
        o5 = o.reshape(b, DIFF_HEADS, 2, l, DIFF_DV).astype(F32)
        d = rmsnorm(o5[:, :, 0] - lam_full * o5[:, :, 1], g_sub) * (1.0 - lam_init)
        return heads_to_tokens(d.astype(o.dtype))

    o = attend_latent(rope_2d(q, rows, cols), q, rope_2d(k, rows, cols), kc, v, vc, scale)
    y_l = combine(o)
    y_c = combine(ctx_attention(qk_heads(q_c), kc, vc, scale)) if with_ctx_out else None
    return y_l, y_c


def mla(cq_l, ckv_l, kpe_l, cq_c, ckv_c, kpe_c, g_q, g_kv, w_uq, w_ukv, rows, cols, with_ctx_out):
    b = cq_l.shape[0]

    def q_heads(cq):
        q = (rmsnorm(cq, g_q) @ w_uq).reshape(b, cq.shape[1], MLA_HEADS, MLA_NOPE + MLA_ROPE).transpose(0, 2, 1, 3)
        return q[..., :MLA_NOPE], q[..., MLA_NOPE:]

    def kv_heads(ckv):
        kv = (rmsnorm(ckv, g_kv) @ w_ukv).reshape(b, ckv.shape[1], MLA_HEADS, MLA_NOPE + MLA_V).transpose(0, 2, 1, 3)
        return kv[..., :MLA_NOPE], kv[..., MLA_NOPE:]

    def with_rope_key(k_nope, k_pe):
        return jnp.concatenate([k_nope, jnp.broadcast_to(k_pe[:, None], k_nope.shape[:3] + (MLA_ROPE,))], axis=-1)

    qn, qp = q_heads(cq_l)
    kn, v = kv_heads(ckv_l)
    knc, vc = kv_heads(ckv_c)
    k_ctx = with_rope_key(knc, kpe_c)
    k_lat = with_rope_key(kn, rope_2d(kpe_l, rows, cols))
    q_lat = jnp.concatenate([qn, rope_2d(qp, rows, cols)], axis=-1)
    q_lc = jnp.concatenate([qn, qp], axis=-1)
    scale = (MLA_NOPE + MLA_ROPE) ** -0.5
    y_l = heads_to_tokens(attend_latent(q_lat, q_lc, k_lat, k_ctx, v, vc, scale))
    y_c = None
    if with_ctx_out:
        qnc, qpc = q_heads(cq_c)
        y_c = heads_to_tokens(ctx_attention(jnp.concatenate([qnc, qpc], axis=-1), k_ctx, vc, scale))
    return y_l, y_c


def _cplx_combine(e1, e2):
    a1r, a1i, b1r, b1i = e1
    a2r, a2i, b2r, b2i = e2
    return (a2r * a1r - a2i * a1i, a2r * a1i + a2i * a1r,
            a2r * b1r - a2i * b1i + b2r, a2r * b1i + a2i * b1r + b2i)


def s5_discretise(lam_re, lam_im, log_step, b_re, b_im):
    lr, li = lam_re.astype(F32), lam_im.astype(F32)
    step = jnp.exp(log_step.astype(F32))[:, None]
    mag = jnp.exp(lr * step)
    ar, ai = mag * jnp.cos(li * step), mag * jnp.sin(li * step)
    den = lr * lr + li * li
    fr = ((ar - 1.0) * lr + ai * li) / den
    fi = (ai * lr - (ar - 1.0) * li) / den
    br, bi = b_re.astype(F32), b_im.astype(F32)
    return ar, ai, fr[..., None] * br - fi[..., None] * bi, fr[..., None] * bi + fi[..., None] * br


def s5_scan(u, disc, h0, reverse):
    ar, ai, bbr, bbi = disc
    br = jnp.einsum('blgj,gpj->lbgp', u, bbr)
    bi = jnp.einsum('blgj,gpj->lbgp', u, bbi)
    if h0 is not None:
        h0r, h0i = h0
        pos = -1 if reverse else 0
        br = br.at[pos].add(ar * h0r - ai * h0i)
        bi = bi.at[pos].add(ar * h0i + ai * h0r)
    a_r = jnp.broadcast_to(ar, br.shape)
    a_i = jnp.broadcast_to(ai, bi.shape)
    _, _, hr, hi = lax.associative_scan(_cplx_combine, (a_r, a_i, br, bi), reverse=reverse, axis=0)
    return hr, hi


def s5_readout(h, c_re, c_im):
    hr, hi = h
    return (jnp.einsum('lbgp,gjp->blgj', hr, c_re.astype(F32))
            - jnp.einsum('lbgp,gjp->blgj', hi, c_im.astype(F32)))


def s5_mixer(u_l, u_c, lam_re, lam_im, log_step, b_re, b_im, c_re, c_im, d, w_glu, b_glu, with_ctx_out):
    b, s, _ = u_l.shape
    lc = u_c.shape[1]
    disc_f = s5_discretise(lam_re[0], lam_im[0], log_step[0], b_re[0], b_im[0])
    disc_b = s5_discretise(lam_re[1], lam_im[1], log_step[1], b_re[1], b_im[1])
    ul = u_l.astype(F32).reshape(b, s, S5_GROUPS, S5_GROUP_CH)
    uc = u_c.astype(F32).reshape(b, lc, S5_GROUPS, S5_GROUP_CH)
    hf_c = s5_scan(uc, disc_f, None, False)
    hb_c = s5_scan(uc, disc_b, None, True)
    hf_l = s5_scan(ul, disc_f, (hf_c[0][-1], hf_c[1][-1]), False)
    hb_l = s5_scan(ul, disc_b, (hb_c[0][0], hb_c[1][0]), True)
    d_f = d.astype(F32)

    def out(hf, hb, u, l):
        y = s5_readout(hf, c_re[0], c_im[0]) + s5_readout(hb, c_re[1], c_im[1]) + d_f * u
        g = jax.nn.gelu(y.reshape(b, l, S5_WIDTH))
        return (g * jax.nn.sigmoid(g @ w_glu.astype(F32) + b_glu.astype(F32))).astype(u_l.dtype)

    y_l = out(hf_l, hb_l, ul, s)
    y_c = out(hf_c, hb_c, uc, lc) if with_ctx_out else None
    return y_l, y_c


def hybrid_mixer(hl, hc, rows, cols, layer, with_ctx_out, w_in, na_rpb, diff_lambda, g_diff, g_mla_q, g_mla_kv,
                 w_mla_uq, w_mla_ukv, s5_lam_re, s5_lam_im, s5_log_step, s5_b_re, s5_b_im, s5_c_re, s5_c_im,
                 s5_d, w_glu, b_glu, w_branch, w_out):
    pl = jnp.split(hl @ w_in, IN_OFFSETS, axis=-1)
    pc = jnp.split(hc @ w_in, IN_OFFSETS, axis=-1)
    lam_init = 0.8 - 0.6 * math.exp(-0.3 * layer)
    ya = neighbourhood_attention(pl[0], pc[0], na_rpb, with_ctx_out)
    yb = diff_attention(pl[1], pl[2], pl[3], pc[1], pc[2], pc[3], diff_lambda, g_diff, lam_init, rows, cols,
                        with_ctx_out)
    ym = mla(pl[4], pl[5], pl[6], pc[4], pc[5], pc[6], g_mla_q, g_mla_kv, w_mla_uq, w_mla_ukv, rows, cols,
             with_ctx_out)
    yd = s5_mixer(pl[7], pc[7], s5_lam_re, s5_lam_im, s5_log_step, s5_b_re, s5_b_im, s5_c_re, s5_c_im, s5_d,
                  w_glu, b_glu, with_ctx_out)
    branches = (ya, yb, ym, yd)

    def merge(ys, gate_pre):
        b, l, _ = gate_pre.shape
        gates = jax.nn.sigmoid(gate_pre.astype(F32).reshape(b, l, N_BRANCH, D_MODEL)).astype(gate_pre.dtype)
        m = gates[:, :, 0] * (ys[0] @ w_branch[0])
        for i in range(1, N_BRANCH):
            m = m + gates[:, :, i] * (ys[i] @ w_branch[i])
        return m @ w_out

    y_l = merge([br[0] for br in branches], pl[8])
    y_c = merge([br[1] for br in branches], pc[8]) if with_ctx_out else None
    return y_l, y_c


def swiglu(x, w1, w3, w2):
    return (jax.nn.silu(x @ w1) * (x @ w3)) @ w2


def routed_experts(h, idx, gw, w_e1, w_e3, w_e2):
    n, d = h.shape
    n_assign = n * TOP_K
    e_flat = idx.reshape(-1)
    t_flat = jnp.repeat(jnp.arange(n, dtype=jnp.int32), TOP_K)
    w_flat = gw.reshape(-1)
    order = jnp.argsort(e_flat)
    e_s, t_s, w_s = e_flat[order], t_flat[order], w_flat[order]
    counts = jnp.bincount(e_flat, length=N_EXPERTS)
    starts = jnp.cumsum(counts) - counts
    padded = (counts + EXPERT_BLOCK - 1) // EXPERT_BLOCK * EXPERT_BLOCK
    pends = jnp.cumsum(padded)
    pstarts = pends - padded
    dest = pstarts[e_s] + jnp.arange(n_assign, dtype=jnp.int32) - starts[e_s]
    n_blocks = -(-n_assign // EXPERT_BLOCK) + N_EXPERTS
    tok = jnp.full((n_blocks * EXPERT_BLOCK,), n, jnp.int32).at[dest].set(t_s)
    wt = jnp.zeros((n_blocks * EXPERT_BLOCK,), h.dtype).at[dest].set(w_s)
    blk_e = jnp.minimum(jnp.searchsorted(pends, jnp.arange(n_blocks, dtype=jnp.int32) * EXPERT_BLOCK,
                                         side='right'), N_EXPERTS - 1)
    h_ext = jnp.concatenate([h, jnp.zeros((1, d), h.dtype)], axis=0)

    def body(acc, blk):
        t, w, e = blk
        y = swiglu(h_ext[t], w_e1[e], w_e3[e], w_e2[e]) * w[:, None]
        return acc.at[t].add(y), None

    acc, _ = lax.scan(body, jnp.zeros_like(h_ext),
                      (tok.reshape(n_blocks, EXPERT_BLOCK), wt.reshape(n_blocks, EXPERT_BLOCK), blk_e))
    return acc[:n]


def moe_ffn(h, w_router, e_bias, w_e1, w_e3, w_e2, w_s1, w_s3, w_s2):
    scores = jax.nn.sigmoid((h @ w_router).astype(F32))
    _, idx = lax.top_k(scores + e_bias.astype(F32), TOP_K)
    gw = jnp.take_along_axis(scores, idx, axis=1)
    gw = gw / jnp.sum(gw, axis=1, keepdims=True) * ROUTED_SCALE
    return swiglu(h, w_s1, w_s3, w_s2) + routed_experts(h, idx, gw.astype(h.dtype), w_e1, w_e3, w_e2)


def setup_inputs(seed: int = 0) -> dict:
    key = jax.random.key(seed)
    ks = iter(jax.random.split(key, 48))
    L, D, G, P, J = DEPTH, D_MODEL, S5_GROUPS, S5_STATE, S5_GROUP_CH
    E, FF = N_EXPERTS, EXPERT_FF

    def nrm(shape, s):
        return jax.random.normal(next(ks), shape, F32) * s

    n_idx = jnp.arange(P, dtype=F32)
    return {
        "x": nrm((BATCH, SEQ, D), 1.0),
        "c": nrm((BATCH, D), 1.0),
        "ctx": nrm((BATCH, CTX_LEN, D), 1.0),
        "c_ctx": nrm((D,), 1.0),
        "w_ada": nrm((L, D, 6 * D), 0.5 * D ** -0.5),
        "b_ada": nrm((L, 6 * D), 0.02),
        "g_norm1": 1.0 + nrm((L, D), 0.02),
        "g_norm2": 1.0 + nrm((L, D), 0.02),
        "w_in": nrm((L, D, IN_W), D ** -0.5),
        "na_rpb": nrm((L, NA_HEADS, 2 * WIN_H - 1, 2 * WIN_W - 1), 0.1),
        "diff_lambda": nrm((L, 4, DIFF_DK), 0.1),
        "g_diff": 1.0 + nrm((L, DIFF_DV), 0.02),
        "g_mla_q": 1.0 + nrm((L, MLA_Q_RANK), 0.02),
        "g_mla_kv": 1.0 + nrm((L, MLA_KV_RANK), 0.02),
        "w_mla_uq": nrm((L, MLA_Q_RANK, MLA_HEADS * (MLA_NOPE + MLA_ROPE)), MLA_Q_RANK ** -0.5),
        "w_mla_ukv": nrm((L, MLA_KV_RANK, MLA_HEADS * (MLA_NOPE + MLA_V)), MLA_KV_RANK ** -0.5),
        "s5_lam_re": -0.5 + nrm((L, 2, G, P), 1e-3),
        "s5_lam_im": math.pi * n_idx + nrm((L, 2, G, P), 1e-3),
        "s5_log_step": jax.random.uniform(next(ks), (L, 2, G), F32, math.log(1e-3), math.log(1e-1)),
        "s5_b_re": nrm((L, 2, G, P, J), (2 * J) ** -0.5),
        "s5_b_im": nrm((L, 2, G, P, J), (2 * J) ** -0.5),
        "s5_c_re": nrm((L, 2, G, J, P), (2 * P) ** -0.5),
        "s5_c_im": nrm((L, 2, G, J, P), (2 * P) ** -0.5),
        "s5_d": nrm((L, G, J), 1.0),
        "w_glu": nrm((L, S5_WIDTH, S5_WIDTH), S5_WIDTH ** -0.5),
        "b_glu": nrm((L, S5_WIDTH), 0.02),
        "w_branch": nrm((L, N_BRANCH, BRANCH_W, D), BRANCH_W ** -0.5),
        "w_out": nrm((L, D, D), D ** -0.5),
        "w_router": nrm((L, D, E), D ** -0.5),
        "e_bias": nrm((L, E), 0.01),
        "w_e1": nrm((L, E, D, FF), D ** -0.5),
        "w_e3": nrm((L, E, D, FF), D ** -0.5),
        "w_e2": nrm((L, E, FF, D), FF ** -0.5),
        "w_s1": nrm((L, D, FF), D ** -0.5),
        "w_s3": nrm((L, D, FF), D ** -0.5),
        "w_s2": nrm((L, FF, D), FF ** -0.5),
        "g_final": 1.0 + nrm((D,), 0.02),
    }


def reference(x, c, ctx, c_ctx, w_ada, b_ada, g_norm1, g_norm2, w_in, na_rpb, diff_lambda, g_diff, g_mla_q,
              g_mla_kv, w_mla_uq, w_mla_ukv, s5_lam_re, s5_lam_im, s5_log_step, s5_b_re, s5_b_im, s5_c_re,
              s5_c_im, s5_d, w_glu, b_glu, w_branch, w_out, w_router, e_bias, w_e1, w_e3, w_e2, w_s1, w_s3,
              w_s2, g_final):
    b, s, d = x.shape
    lc = ctx.shape[1]
    pos = jnp.arange(s, dtype=jnp.int32)
    rows, cols = pos // GRID_W, pos % GRID_W
    xl, xc = x, ctx
    sc, scc = jax.nn.silu(c), jax.nn.silu(c_ctx)
    for l in range(DEPTH):
        ctx_out = l < DEPTH - 1
        sh1, sc1, gt1, sh2, sc2, gt2 = jnp.split((sc @ w_ada[l] + b_ada[l])[:, None, :], 6, axis=-1)
        csh1, csc1, cgt1, csh2, csc2, cgt2 = jnp.split((scc @ w_ada[l] + b_ada[l])[None, None, :], 6, axis=-1)
        hl = modulate(rmsnorm(xl, g_norm1[l]), sh1, sc1)
        hc = modulate(rmsnorm(xc, g_norm1[l]), csh1, csc1)
        yl, yc = hybrid_mixer(hl, hc, rows, cols, l, ctx_out, w_in[l], na_rpb[l], diff_lambda[l], g_diff[l],
                              g_mla_q[l], g_mla_kv[l], w_mla_uq[l], w_mla_ukv[l], s5_lam_re[l], s5_lam_im[l],
                              s5_log_step[l], s5_b_re[l], s5_b_im[l], s5_c_re[l], s5_c_im[l], s5_d[l],
                              w_glu[l], b_glu[l], w_branch[l], w_out[l])
        xl = xl + gt1 * yl
        hl2 = modulate(rmsnorm(xl, g_norm2[l]), sh2, sc2)
        moe_w = (w_router[l], e_bias[l], w_e1[l], w_e3[l], w_e2[l], w_s1[l], w_s3[l], w_s2[l])
        if ctx_out:
            xc = xc + cgt1 * yc
            hc2 = modulate(rmsnorm(xc, g_norm2[l]), csh2, csc2)
            f = moe_ffn(jnp.concatenate([hl2.reshape(-1, d), hc2.reshape(-1, d)], axis=0), *moe_w)
            xl = xl + gt2 * f[:b * s].reshape(b, s, d)
            xc = xc + cgt2 * f[b * s:].reshape(b, lc, d)
        else:
            xl = xl + gt2 * moe_ffn(hl2.reshape(-1, d), *moe_w).reshape(b, s, d)
    return rmsnorm(xl, g_final)
```

```python
import math
from contextlib import ExitStack
import numpy as np
import concourse.bass as bass
import concourse.mybir as mybir
from concourse.bass_utils import run_bass_kernel_spmd

F32 = mybir.dt.float32
BF16 = mybir.dt.bfloat16
I32 = mybir.dt.int32
AF = mybir.ActivationFunctionType
ALU = mybir.AluOpType
AX = mybir.AxisListType

D = 1024
NCTX = 256
GW = 64
INW = 6304
EPS = 1e-6
TWO_PI = 2.0 * math.pi


class Buf:
    __slots__ = ("w", "r", "x")

    def __init__(self, x=False):
        self.w = None
        self.r = {}
        self.x = x


class TK:
    NDS = 8

    def __init__(self, nc):
        self.nc = nc
        self.engs = {"pe": nc.tensor, "act": nc.scalar, "dve": nc.vector, "pool": nc.gpsimd, "sp": nc.sync}
        self.sem = {e: nc.alloc_semaphore(name=f"s_{e}") for e in ("pe", "act", "dve", "pool")}
        self.cnt = {e: 0 for e in self.sem}
        self.waited = {e: {} for e in self.engs}
        self.dsem = {q: [nc.alloc_semaphore(name=f"d_{q}{i}") for i in range(self.NDS)] for q in ("sp", "pool")}
        self.dcnt = {q: [0] * self.NDS for q in self.dsem}
        self.dnext = {q: 0 for q in self.dsem}
        self.pending = {}
        self.nins = 0

    def _need(self, r, w):
        need = {}

        def add(d):
            if d is None:
                return
            k, s, v = d
            if k not in need or need[k][1] < v:
                need[k] = (s, v)
        for b in r:
            add(b.w)
            if b.x:
                for d in b.r.values():
                    add(d)
        for b in w:
            add(b.w)
            for d in b.r.values():
                add(d)
        return need

    def _wait(self, e, need):
        E = self.engs[e]
        nw = 0
        for k, (s, v) in need.items():
            if k == "pe" and e == "pe":
                continue
            if self.waited[e].get(k, 0) >= v:
                continue
            E.wait_ge(s, v)
            nw += 1
            self.waited[e][k] = v

    def _mark(self, d, r, w):
        for b in r:
            b.r[d[0]] = d
        for b in w:
            b.w = d
            b.r = {}

    def op(self, e, fn, r=(), w=()):
        r = [x.b if hasattr(x, "b") else x for x in r]
        w = [x.b if hasattr(x, "b") else x for x in w]
        self._wait(e, self._need(r, w))
        ins = fn(self.engs[e])
        self.cnt[e] += 1
        self.nins += 1
        ins.then_inc(self.sem[e], 1)
        self._mark((e, self.sem[e], self.cnt[e]), r, w)
        return ins

    def dma(self, q, out, in_, r=(), w=(), **kw):
        r = [x.b if hasattr(x, "b") else x for x in r]
        w = [x.b if hasattr(x, "b") else x for x in w]
        self._wait(q, self._need(r, w))
        E = self.engs[q]
        i = self.dnext[q]
        self.dnext[q] = (i + 1) % self.NDS
        s = self.dsem[q][i]
        prev = self.dcnt[q][i]
        key = f"d{q}{i}"
        if prev > 0 and self.waited[q].get(key, 0) < prev:
            E.wait_ge(s, prev)
            self.waited[q][key] = prev
        ins = E.dma_start(out=out, in_=in_, **kw)
        ins.then_inc(s, 16)
        self.nins += 1
        self.dcnt[q][i] = prev + 16
        d = (key, s, prev + 16)
        self.pending[key] = d
        self._mark(d, r, w)
        return d

    def barrier(self, engines=("pe", "act", "dve", "pool", "sp")):
        need = {e: (self.sem[e], self.cnt[e]) for e in self.sem if self.cnt[e] > 0}
        for k, d in self.pending.items():
            need[k] = (d[1], d[2])
        for e in engines:
            E = self.engs[e]
            for k, (s, v) in need.items():
                if k == e:
                    continue
                if self.waited[e].get(k, 0) >= v:
                    continue
                E.wait_ge(s, v)
                self.waited[e][k] = v
        self.pending = {}


class T:
    def __init__(self, t, x=False):
        self.t = t
        self.b = Buf(x)

    def __getitem__(self, k):
        return self.t[k]


class Rot:
    def __init__(self, tiles):
        self.tiles = tiles
        self.i = 0

    def get(self):
        t = self.tiles[self.i]
        self.i = (self.i + 1) % len(self.tiles)
        return t


class KB:
    def __init__(self, SEQ, DEPTH, dbg=None):
        self.SEQ, self.DEPTH = SEQ, DEPTH
        self.NT = SEQ
        self.TL = SEQ + NCTX
        self.NTI = self.TL // 128
        self.NLT = SEQ // 128
        self.ROWS = SEQ // GW
        self.NCH = self.TL // 8
        self.dbg = dbg or []
        self.nc = bass.Bass("TRN2", target_bir_lowering=False)
        self.tk = TK(self.nc)
        self.inp = {}
        self.uid = 0

    def I(self, name, shape, dt=F32):
        self.inp[name] = self.nc.dram_tensor(name, list(shape), dt, kind="ExternalInput").ap()
        return self.inp[name]

    def DR(self, name, shape, dt=BF16):
        t = T(self.nc.dram_tensor(name, list(shape), dt).ap())
        return t

    def sb(self, es, shape, dt=BF16, name=None):
        self.uid += 1
        return T(es.enter_context(self.nc.sbuf_tensor(f"{name or 'sb'}_{self.uid}", list(shape), dt)))

    def rot(self, es, n, shape, dt=BF16, name=None):
        return Rot([self.sb(es, shape, dt, name) for _ in range(n)])

    def psb(self):
        return self.psr.get()

    def op(self, e, fn, r=(), w=()):
        return self.tk.op(e, fn, r, w)

    def ld(self, out, in_, r=(), w=(), **kw):
        return self.tk.dma("sp", out, in_, r, w, **kw)

    def st(self, out, in_, r=(), w=(), **kw):
        return self.tk.dma("pool", out, in_, r, w, **kw)

    def mm(self, ps, out, lhsT, rhs, start, stop, r=()):
        return self.op("pe", lambda e: e.matmul(out, lhsT=lhsT, rhs=rhs, start=start, stop=stop), r=r, w=[ps])

    def load_cast(self, es, dst, dst_ap, src_ap, shape, eng="dve"):
        stg = self.stage.get()
        view = stg.t[:]
        n = 1
        for s in shape[1:]:
            n *= s
        flat = stg.t[0:shape[0], 0:n]
        if len(shape) == 3:
            flat = flat.rearrange("p (a b) -> p a b", a=shape[1])
        self.ld(flat, src_ap, w=[stg])
        self.op(eng, lambda e: e.tensor_copy(out=dst_ap, in_=flat), r=[stg], w=[dst])


def _build(SEQ, DEPTH, dbg_names=()):
    k = KB(SEQ, DEPTH)
    nc, tk = k.nc, k.tk
    NT, TL, NTI, NLT, ROWS = k.NT, k.TL, k.NTI, k.NLT, k.ROWS
    L = DEPTH
    x_in = k.I("x", [NT, D])
    ctx_in = k.I("ctx", [NCTX, D])
    cT_in = k.I("cT", [128, 8, 2])
    w_ada = k.I("w_ada", [L, D, 6 * D])
    b_adaT = k.I("b_adaT", [L, 128, 48])
    g1T = k.I("g1T", [L, 128, 8])
    g2T = k.I("g2T", [L, 128, 8])
    w_in = k.I("w_in", [L, D, INW])
    w_uq = k.I("w_uq", [L, 256, 384])
    w_ukv = k.I("w_ukv", [L, 128, 512])
    g_mq = k.I("g_mla_q", [L, 256])
    g_mkv = k.I("g_mla_kv", [L, 128])
    ropeC = k.I("ropeC", [128, TL])
    ropeS = k.I("ropeS", [128, TL])
    nab = k.I("nab", [L, 128, 4 * 14 * 64])
    g_diff = k.I("g_diff", [L, 64])
    dlam = k.I("diff_lambda", [L, 128])
    lamc = k.I("lamc", [64, 2])
    w_branch = k.I("w_branch", [L, 4, 256, D])
    w_out = k.I("w_out", [L, D, D])
    w_router = k.I("w_router", [L, D, 64])
    e_bias = k.I("e_bias", [L, 64])
    w_e1 = k.I("w_e1", [L, 64, D, 256])
    w_e3 = k.I("w_e3", [L, 64, D, 256])
    w_e2 = k.I("w_e2", [L, 64, 256, D])
    w_s1 = k.I("w_s1", [L, D, 256])
    w_s3 = k.I("w_s3", [L, D, 256])
    w_s2 = k.I("w_s2", [L, 256, D])
    g_final = k.I("g_final", [D])
    s5p = k.I("s5p", [L, 2, 128, 3 * 16])
    s5B = k.I("s5B", [L, 2, 2, 128, 256])
    s5C = k.I("s5C", [L, 2, 2, 128, 256])
    s5kv = k.I("s5kv", [128, 6 * 8])
    s5msk = k.I("s5msk", [2, 128, 128])
    s5J = k.I("s5J", [128, 128])
    s5sg = k.I("s5sg", [128, 2])
    s5_d = k.I("s5_d", [L, 256])
    w_glu = k.I("w_glu", [L, 256, 256])
    b_glu = k.I("b_glu", [L, 256])
    y_out = nc.dram_tensor("y", [NT, D], F32, kind="ExternalOutput").ap()
    x_out = nc.dram_tensor("x_out", [NT, D], F32, kind="ExternalOutput").ap()
    ctx_out = nc.dram_tensor("ctx_out", [NCTX, D], F32, kind="ExternalOutput").ap()
    dbg = {}
    for nm, shp in dbg_names:
        dbg[nm] = nc.dram_tensor(nm, list(shp), F32, kind="ExternalOutput").ap()

    X = k.DR("X", [TL, D], F32)
    HT = k.DR("HT", [D, TL])
    QR = k.DR("QR", [896, TL])
    QP = k.DR("QP", [896, TL])
    KT = k.DR("KT", [896, TL])
    V = k.DR("V", [TL, 768])
    UT = k.DR("UT", [TL, 256], F32)
    YS = k.DR("YS", [TL, 256], F32)
    YT = k.DR("YT", [D, TL])
    H2T = k.DR("H2T", [D, TL])
    GM = k.DR("GM", [TL, 65], F32)
    EW = k.DR("EW", [65, 128, 6144])
    gts = k.DR("gts", [4, D], F32)
    lamsc = k.DR("lamsc", [1, 64], F32)

    with ExitStack() as top:
        k.psr = Rot([T(top.enter_context(nc.psum_tensor(f"ps{i}", [128, 512], F32)), x=True) for i in range(8)])
        identf = k.sb(top, [128, 128], F32, "identf")
        identb = k.sb(top, [128, 128], BF16, "identb")
        onesf = k.sb(top, [128, 128], F32, "onesf")
        epsT = k.sb(top, [128, 1], F32, "eps")
        scT = k.sb(top, [128, 8, 2], F32, "scT")
        k.op("pool", lambda e: e.memset(identf[:], 1.0), w=[identf])
        k.op("pool", lambda e: e.affine_select(out=identf[:], in_=identf[:], pattern=[[-1, 128]], compare_op=ALU.is_equal,
                                               fill=0.0, base=0, channel_multiplier=1), r=[identf], w=[identf])
        k.op("dve", lambda e: e.tensor_copy(out=identb[:], in_=identf[:]), r=[identf], w=[identb])
        k.op("pool", lambda e: e.memset(onesf[:], 1.0), w=[onesf])
        k.op("pool", lambda e: e.memset(epsT[:], EPS), w=[epsT])
        k.ld(scT[:], cT_in[:, :, :], w=[scT])
        k.op("act", lambda e: e.activation(out=scT[:], in_=scT[:], func=AF.Silu), r=[scT], w=[scT])
        k.st(X[0:NT, :], x_in[:, :], w=[X])
        k.st(X[NT:TL, :], ctx_in[:, :], w=[X])
        tk.barrier()

        for l in range(L):
            layer(k, l, locals())
        final_norm(k, X, g_final, y_out, epsT)
        k.st(x_out[:, :], X[0:NT, :], r=[X])
        k.st(ctx_out[:, :], X[NT:TL, :], r=[X])
        tk.barrier()
    return k


def rstd_from_ss(k, out, ss, n, epsT, r=(), w=()):
    P = out.shape[0] if hasattr(out, "shape") else 128
    k.op("act", lambda e: e.activation(out=out, in_=ss, func=AF.Sqrt, scale=1.0 / n, bias=epsT), r=r, w=w)
    k.op("dve", lambda e: e.reciprocal(out=out, in_=out), r=w, w=w)


def final_norm(k, X, g_final, y_out, epsT):
    nc = k.nc
    with ExitStack() as es:
        gf = k.sb(es, [128, D], F32)
        k.ld(gf[:], g_final.rearrange("(o d) -> o d", o=1).partition_broadcast(128), w=[gf])
        xr = k.rot(es, 2, [128, D], F32)
        jr = k.rot(es, 2, [128, D], F32)
        sr = k.rot(es, 2, [128, 1], F32)
        for t in range(k.NLT):
            xt, jt, ss = xr.get(), jr.get(), sr.get()
            k.ld(xt[:], X[t * 128:(t + 1) * 128, :], r=[X], w=[xt])
            k.op("act", lambda e: e.activation(out=jt[:], in_=xt[:], func=AF.Square, accum_out=ss[:]), r=[xt], w=[jt, ss])
            rstd_from_ss(k, ss[:], ss[:], D, epsT[:], r=[ss], w=[ss])
            k.op("dve", lambda e: e.scalar_tensor_tensor(out=jt[:], in0=xt[:], scalar=ss[:, 0:1], in1=gf[:], op0=ALU.mult,
                                                        op1=ALU.mult), r=[xt, ss, gf], w=[jt])
            k.st(y_out[t * 128:(t + 1) * 128, :], jt[:], r=[jt])


def layer(k, l, G):
    import os
    from types import SimpleNamespace
    g = SimpleNamespace(**G)
    STOP = int(os.environ.get('KSTOP', '99'))
    if STOP >= 1:
        phase_mod(k, l, g)
    else:
        k.lay = ExitStack()
    if STOP >= 2:
        phase_proj(k, l, g)
    k.tk.barrier()
    phase_attn(k, l, g)
    phase_s5(k, l, g)
    k.tk.barrier()
    phase_merge(k, l, g)
    k.tk.barrier()
    phase_moe(k, l, g)
    k.tk.barrier()


def phase_mod(k, l, g):
    nc = k.nc
    es = k.lay = ExitStack()
    k.A1 = k.sb(es, [128, 8, 2], F32)
    k.B1 = k.sb(es, [128, 8, 2], F32)
    k.A2 = k.sb(es, [128, 8, 2], F32)
    k.B2 = k.sb(es, [128, 8, 2], F32)
    modT = k.sb(es, [128, 48, 2], F32)
    with ExitStack() as s2:
        war = k.rot(s2, 2, [128, 8, 768], F32)
        bT = k.sb(s2, [128, 48], F32)
        gT = k.sb(s2, [128, 16], F32)
        k.ld(bT[:], g.b_adaT[l], w=[bT])
        k.ld(gT[:, 0:8], g.g1T[l], w=[gT])
        k.ld(gT[:, 8:16], g.g2T[l], w=[gT])
        ps = k.psb()
        for og in range(8):
            wa = war.get()
            k.ld(wa[:], g.w_ada[l, :, og * 768:(og + 1) * 768].rearrange("(kc p) c -> p kc c", p=128), w=[wa])
            for oc in range(6):
                o = (og * 6 + oc) * 2
                for kc in range(8):
                    k.mm(ps, ps[:, o:o + 2], wa[:, kc, oc * 128:(oc + 1) * 128], g.scT[:, kc, :], kc == 0, kc == 7, r=[wa, g.scT])
        k.op("dve", lambda e: e.tensor_tensor(out=modT[:], in0=ps[:, 0:96].rearrange("p (a b) -> p a b", b=2),
                                             in1=bT[:].unsqueeze(2).broadcast_to([128, 48, 2]), op=ALU.add), r=[ps, bT], w=[modT])
        for (A, B, gi, so, sh) in ((k.A1, k.B1, 0, 8, 0), (k.A2, k.B2, 8, 32, 24)):
            k.op("dve", lambda e: e.tensor_scalar(out=A[:], in0=modT[:, so:so + 8, :], scalar1=1.0, scalar2=None, op0=ALU.add),
                 r=[modT], w=[A])
            k.op("dve", lambda e: e.tensor_tensor(out=A[:], in0=A[:], in1=gT[:, gi:gi + 8].unsqueeze(2).broadcast_to([128, 8, 2]),
                                                 op=ALU.mult), r=[A, gT], w=[A])
            k.op("dve", lambda e: e.tensor_copy(out=B[:], in_=modT[:, sh:sh + 8, :]), r=[modT], w=[B])
        for kind, off in ((0, 16), (1, 40)):
            for b in range(2):
                k.st(g.gts[kind * 2 + b, :].rearrange("(j p) -> p j", p=128), modT[:, off:off + 8, b], r=[modT], w=[g.gts], allow_slow_non_contiguous=True)
        k.tk.barrier()
    k.GT = [[k.sb(es, [128, D], F32) for b in range(2)] for kind in range(2)]
    for kind in range(2):
        for b in range(2):
            k.ld(k.GT[kind][b][:], g.gts[kind * 2 + b:kind * 2 + b + 1, :].partition_broadcast(128), r=[g.gts], w=[k.GT[kind][b]])


def norm_mod_T(k, es_tiles, xt, A, B, b, epsT, identb, hT):
    jt, ss, xn = es_tiles
    k.op("act", lambda e: e.activation(out=jt[:], in_=xt[:], func=AF.Square, accum_out=ss[:]), r=[xt], w=[jt, ss])
    rstd_from_ss(k, ss[:], ss[:], D, epsT[:], r=[ss], w=[ss])
    k.op("act", lambda e: e.activation(out=xn[:], in_=xt[:], func=AF.Copy, scale=ss[:, 0:1]), r=[xt, ss], w=[xn])
    ps = k.psb()
    pv = ps[:].bitcast(BF16)
    for kc in range(8):
        k.op("pe", lambda e: e.transpose(pv[:, kc * 128:(kc + 1) * 128], xn[:, kc * 128:(kc + 1) * 128], identb[:]), r=[xn, identb], w=[ps])
    for kc in range(8):
        k.op("dve", lambda e: e.tensor_scalar(out=hT[:, kc, :], in0=pv[:, kc * 128:(kc + 1) * 128], scalar1=A[:, kc, b:b + 1],
                                             scalar2=B[:, kc, b:b + 1], op0=ALU.mult, op1=ALU.add), r=[ps, A, B], w=[hT])


def perm_copy(k, dst, src, r, w):
    sv = src.rearrange("p a (b t e) -> p a b t e", t=2, e=8)
    dv = dst.rearrange("p a (b t e) -> p a b t e", t=2, e=8)
    k.op("pool", lambda e: e.tensor_copy(out=dv[:, :, :, 0, :], in_=sv[:, :, :, 1, :]), r=r, w=w)
    k.op("pool", lambda e: e.tensor_copy(out=dv[:, :, :, 1, :], in_=sv[:, :, :, 0, :]), r=r, w=w)


def phase_proj(k, l, g):
    nc = k.nc
    NT, TL, NTI = k.NT, k.TL, k.NTI
    with ExitStack() as es:
        k.stage = k.rot(es, 2, [128, 8 * 768], F32, "stage")
        wi = k.sb(es, [128, 8, 2208], BF16, "wi")
        for c0 in range(0, 2208, 736):
            k.load_cast(es, wi, wi[:, :, c0:c0 + 736], g.w_in[l, :, c0:c0 + 736].rearrange("(kc p) c -> p kc c", p=128), [128, 8, 736])
        wip = k.sb(es, [128, 8, 544], BF16, "wip")
        perm_copy(k, wip[:, :, 0:512], wi[:, :, 768:1280], [wi], [wip])
        perm_copy(k, wip[:, :, 512:544], wi[:, :, 1920:1952], [wi], [wip])
        wuq = k.sb(es, [128, 2, 384], BF16, "wuq")
        k.load_cast(es, wuq, wuq[:], g.w_uq[l].rearrange("(kc p) c -> p kc c", p=128), [128, 2, 384])
        wuqp = k.sb(es, [128, 2, 384], BF16, "wuqp")
        k.op("pool", lambda e: e.tensor_copy(out=wuqp[:], in_=wuq[:]), r=[wuq], w=[wuqp])
        for h in range(4):
            perm_copy(k, wuqp[:, :, h * 96 + 64:h * 96 + 96], wuq[:, :, h * 96 + 64:h * 96 + 96], [wuq], [wuqp])
        wkv = k.sb(es, [128, 1, 512], BF16, "wkv")
        k.load_cast(es, wkv, wkv[:], g.w_ukv[l].rearrange("(o p) c -> p o c", o=1), [128, 1, 512])
        wkvv = k.sb(es, [128, 4, 64], BF16, "wkvv")
        k.op("pool", lambda e: e.tensor_copy(out=wkvv[:], in_=wkv[:, 0, :].rearrange("p (h t e) -> p h t e", h=4, t=2)[:, :, 1, :]),
             r=[wkv], w=[wkvv])
        gq = k.sb(es, [128, 384], F32, "gq")
        k.ld(gq[:, 0:256], g.g_mq[l:l + 1, :].partition_broadcast(128), w=[gq])
        k.ld(gq[:, 256:384], g.g_mkv[l:l + 1, :].partition_broadcast(128), w=[gq])
        xr = k.rot(es, 2, [128, D], F32, "x")
        jr = k.rot(es, 2, [128, D], F32, "j")
        sr = k.rot(es, 2, [128, 1], F32, "ss")
        xnr = k.rot(es, 2, [128, D], BF16, "xn")
        hr = k.rot(es, 2, [128, 8, 128], BF16, "hT")
        cr_ = k.rot(es, 2, [128, 128], F32, "rc")
        sr_ = k.rot(es, 2, [128, 128], F32, "rs")
        ob = k.rot(es, 6, [128, 128], BF16, "ob")
        t1r = k.rot(es, 3, [128, 128], F32, "t1")
        t2r = k.rot(es, 3, [128, 128], F32, "t2")
        vb = k.rot(es, 2, [128, 768], BF16, "vb")
        ur = k.rot(es, 2, [128, 256], F32, "u")
        mr = k.rot(es, 2, [128, 384], F32, "m")
        mbr = k.rot(es, 2, [128, 384], BF16, "mb")
        mtr = k.rot(es, 2, [128, 3, 128], BF16, "mt")
        s2r = k.rot(es, 2, [128, 2], F32, "s2")
        import os
        for t in range(NTI if int(os.environ.get('KSTOP', '99')) >= 3 else 0):
            b = 0 if t < k.NLT else 1
            cs = slice(t * 128, (t + 1) * 128)
            xt, hT = xr.get(), hr.get()
            k.ld(xt[:], g.X[cs, :], r=[g.X], w=[xt])
            norm_mod_T(k, (jr.get(), sr.get(), xnr.get()), xt, k.A1, k.B1, b, g.epsT, g.identb, hT)
            SUB = int(os.environ.get('KSUB', '99'))
            if SUB < 1:
                continue
            k.st(g.HT[:, cs].rearrange("(kc p) t -> p kc t", p=128), hT[:], r=[hT], w=[g.HT])
            if SUB < 2:
                continue
            rc, rs = cr_.get(), sr_.get()
            k.ld(rc[:], g.ropeC[:, cs], w=[rc])
            k.ld(rs[:], g.ropeS[:, cs], w=[rs])

            def fm(c0, n, wt=wi):
                ps = k.psb()
                for kc in range(8):
                    k.mm(ps, ps[0:n, 0:128], wt[:, kc, c0:c0 + n], hT[:, kc, :], kc == 0, kc == 7, r=[wt, hT])
                return ps

            def emit(ps, n, dsts, p0=0):
                o = ob.get()
                k.op("act", lambda e: e.activation(out=o[p0:p0 + n, :], in_=ps[p0:p0 + n, 0:128], func=AF.Copy), r=[ps], w=[o])
                for (dt_, r0) in dsts:
                    k.st(dt_[r0:r0 + n, cs], o[p0:p0 + n, :], r=[o], w=[dt_])
                k.last_o = o

            def emit_rope(ps, pp, n, dsts, p0=0):
                t1, t2, o = t1r.get(), t2r.get(), ob.get()
                sl = slice(p0, p0 + n)
                RV = int(os.environ.get('KRV', '99'))
                if RV == -1:
                    k.op("dve", lambda e: e.tensor_copy(out=t1[sl, :], in_=ps[sl, 0:128]), r=[ps, k.last_o], w=[t1])
                elif RV == -2:
                    k.op("dve", lambda e: e.tensor_tensor(out=t1[sl, :], in0=rc[sl, :], in1=rc[sl, :], op=ALU.mult), r=[rc], w=[t1])
                else:
                    k.op("dve", lambda e: e.tensor_tensor(out=t1[sl, :], in0=ps[sl, 0:128], in1=rc[sl, :], op=ALU.mult), r=[ps, rc], w=[t1])
                if RV >= 1:
                    k.op("dve", lambda e: e.tensor_tensor(out=t2[sl, :], in0=pp[sl, 0:128], in1=rs[sl, :], op=ALU.mult), r=[pp, rs], w=[t2])
                if RV >= 2:
                    k.op("dve", lambda e: e.tensor_tensor(out=o[sl, :], in0=t1[sl, :], in1=t2[sl, :], op=ALU.add), r=[t1, t2], w=[o])
                else:
                    k.op("act", lambda e: e.activation(out=o[sl, :], in_=t1[sl, :], func=AF.Copy), r=[t1], w=[o])
                for (dt_, r0) in dsts:
                    k.st(dt_[r0:r0 + n, cs], o[sl, :], r=[o], w=[dt_])
            for i in range(2):
                emit(fm(i * 128, 128), 128, [(g.QR, i * 128), (g.QP, i * 128)])
                emit(fm(256 + i * 128, 128), 128, [(g.KT, i * 128)])
            if SUB < 3:
                continue
            S2 = int(os.environ.get('KSUB2', '99'))
            for i in range(2):
                ps = fm(768 + i * 128, 128)
                pp = fm(i * 128, 128, wip)
                emit(ps, 128, [(g.QP, 256 + i * 128)])
                if S2 < 1:
                    emit(pp, 128, [(g.QR, 256 + i * 128)])
                    continue
                emit_rope(ps, pp, 128, [(g.QR, 256 + i * 128)])
                if S2 < 2:
                    continue
                ps = fm(1024 + i * 128, 128)
                pp = fm(256 + i * 128, 128, wip)
                emit_rope(ps, pp, 128, [(g.KT, 256 + i * 128)])
            if S2 >= 3:
                ps = fm(1920, 32)
                pp = fm(512, 32, wip)
                emit_rope(ps, pp, 32, [(g.KT, 512 + h * 96 + 64) for h in range(4)])
            if SUB < 4:
                continue
            vt = vb.get()
            for (c0, o0) in ((512, 0), (1280, 256)):
                ps = k.psb()
                for kc in range(8):
                    k.mm(ps, ps[:, 0:256], hT[:, kc, :], wi[:, kc, c0:c0 + 256], kc == 0, kc == 7, r=[wi, hT])
                k.op("act", lambda e: e.activation(out=vt[:, o0:o0 + 256], in_=ps[:, 0:256], func=AF.Copy), r=[ps], w=[vt])
            ps = k.psb()
            for kc in range(8):
                k.mm(ps, ps[:, 0:256], hT[:, kc, :], wi[:, kc, 1952:2208], kc == 0, kc == 7, r=[wi, hT])
            ut = ur.get()
            k.op("act", lambda e: e.activation(out=ut[:], in_=ps[:, 0:256], func=AF.Copy), r=[ps], w=[ut])
            k.st(g.UT[cs, :], ut[:], r=[ut], w=[g.UT])
            if SUB < 5:
                continue
            ps = k.psb()
            for kc in range(8):
                k.mm(ps, ps[:, 0:384], hT[:, kc, :], wi[:, kc, 1536:1920], kc == 0, kc == 7, r=[wi, hT])
            m, mb_, s2, mt = mr.get(), mbr.get(), s2r.get(), mtr.get()
            k.op("act", lambda e: e.activation(out=m[:, 0:256], in_=ps[:, 0:256], func=AF.Square, accum_out=s2[:, 0:1]), r=[ps], w=[m, s2])
            k.op("act", lambda e: e.activation(out=m[:, 256:384], in_=ps[:, 256:384], func=AF.Square, accum_out=s2[:, 1:2]), r=[ps], w=[m, s2])
            rstd_from_ss(k, s2[:, 0:1], s2[:, 0:1], 256, g.epsT[:], r=[s2], w=[s2])
            rstd_from_ss(k, s2[:, 1:2], s2[:, 1:2], 128, g.epsT[:], r=[s2], w=[s2])
            k.op("dve", lambda e: e.scalar_tensor_tensor(out=mb_[:, 0:256], in0=ps[:, 0:256], scalar=s2[:, 0:1], in1=gq[:, 0:256],
                                                        op0=ALU.mult, op1=ALU.mult), r=[ps, s2, gq], w=[mb_])
            k.op("dve", lambda e: e.scalar_tensor_tensor(out=mb_[:, 256:384], in0=ps[:, 256:384], scalar=s2[:, 1:2], in1=gq[:, 256:384],
                                                        op0=ALU.mult, op1=ALU.mult), r=[ps, s2, gq], w=[mb_])
            pt = k.psb()
            ptv = pt[:].bitcast(BF16)
            for i in range(3):
                k.op("pe", lambda e: e.transpose(ptv[:, i * 128:(i + 1) * 128], mb_[:, i * 128:(i + 1) * 128], g.identb[:]), r=[mb_, g.identb], w=[pt])
            k.op("act", lambda e: e.activation(out=mt[:].rearrange("p a b -> p (a b)"), in_=ptv[:, 0:384], func=AF.Copy), r=[pt], w=[mt])
            for h in range(4):
                ps = k.psb()
                pp = k.psb()
                for kc in range(2):
                    k.mm(ps, ps[0:96, 0:128], wuq[:, kc, h * 96:(h + 1) * 96], mt[:, kc, :], kc == 0, kc == 1, r=[wuq, mt])
                for kc in range(2):
                    k.mm(pp, pp[0:96, 0:128], wuqp[:, kc, h * 96:(h + 1) * 96], mt[:, kc, :], kc == 0, kc == 1, r=[wuqp, mt])
                emit(ps, 96, [(g.QP, 512 + h * 96)])
                emit(ps, 64, [(g.QR, 512 + h * 96)])
                emit_rope(ps, pp, 32, [(g.QR, 512 + h * 96 + 64 - 64)], p0=64) if False else None
                t1, t2, o = t1r.get(), t2r.get(), ob.get()
                sl = slice(64, 96)
                k.op("dve", lambda e: e.tensor_tensor(out=t1[sl, :], in0=ps[sl, 0:128], in1=rc[sl, :], op=ALU.mult), r=[ps, rc], w=[t1])
                k.op("dve", lambda e: e.tensor_tensor(out=t2[sl, :], in0=pp[sl, 0:128], in1=rs[sl, :], op=ALU.mult), r=[pp, rs], w=[t2])
                k.op("dve", lambda e: e.tensor_tensor(out=o[sl, :], in0=t1[sl, :], in1=t2[sl, :], op=ALU.add), r=[t1, t2], w=[o])
                k.st(g.QR[512 + h * 96 + 64:512 + h * 96 + 96, cs], o[sl, :], r=[o], w=[g.QR])
                ps = k.psb()
                k.mm(ps, ps[0:64, 0:128], wkv[:, 0, h * 128:h * 128 + 64], mt[:, 2, :], True, True, r=[wkv, mt])
                emit(ps, 64, [(g.KT, 512 + h * 96)])
            ps = k.psb()
            k.mm(ps, ps[:, 0:256], mt[:, 2, :], wkvv[:].rearrange("p h e -> p (h e)"), True, True, r=[wkvv, mt])
            k.op("act", lambda e: e.activation(out=vt[:, 512:768], in_=ps[:, 0:256], func=AF.Copy), r=[ps], w=[vt])
            k.st(g.V[cs, :], vt[:], r=[vt], w=[g.V])


def epilogue(k, g, po, n, osr, onr):
    osb, on = osr.get(), onr.get()
    k.op("act", lambda e: e.activation(out=osb[0:65, 0:n], in_=po[0:65, 0:n], func=AF.Copy), r=[po], w=[osb])
    k.op("dve", lambda e: e.reciprocal(out=osb[64:65, 0:n], in_=osb[64:65, 0:n]), r=[osb], w=[osb])
    pb = k.pss.get()
    k.mm(pb, pb[0:64, 0:n], g.onesf[64:65, 0:64], osb[64:65, 0:n], True, True, r=[osb, g.onesf])
    k.op("dve", lambda e: e.tensor_tensor(out=on[0:64, 0:n], in0=osb[0:64, 0:n], in1=pb[0:64, 0:n], op=ALU.mult), r=[osb, pb], w=[on])
    return on


def phase_attn(k, l, g):
    NT, TL, NLT = k.NT, k.TL, k.NLT
    NKT = TL // 128
    ROWS = k.ROWS
    k.pss = Rot(k.psr.tiles[0:5])
    k.pso = Rot(k.psr.tiles[5:8])
    with ExitStack() as es:
        nabb = k.sb(es, [128, 4, 14, 64], BF16, "nabb")
        with ExitStack() as s2:
            nf = k.sb(s2, [128, 3584], F32, "nabf")
            k.ld(nf[:], g.nab[l], w=[nf])
            k.op("dve", lambda e: e.tensor_scalar(out=nabb[:].rearrange("p a b c -> p (a b c)"), in0=nf[:], scalar1=8.0, scalar2=None,
                                                 op0=ALU.mult), r=[nf], w=[nabb])
            k.tk.barrier()
        dl = k.sb(es, [64, 128], F32, "dl")
        lam = k.sb(es, [64, 4], F32, "lam")
        gd = k.sb(es, [64, 1], F32, "gd")
        k.ld(dl[:], g.dlam[l:l + 1, :].partition_broadcast(64), w=[dl])
        k.ld(gd[:], g.g_diff[l, :].rearrange("(d o) -> d o", o=1), w=[gd])
        k.op("dve", lambda e: e.tensor_tensor(out=dl[:, 0:32], in0=dl[:, 0:32], in1=dl[:, 32:64], op=ALU.mult), r=[dl], w=[dl])
        k.op("dve", lambda e: e.tensor_tensor(out=dl[:, 64:96], in0=dl[:, 64:96], in1=dl[:, 96:128], op=ALU.mult), r=[dl], w=[dl])
        k.op("act", lambda e: e.activation(out=dl[:, 32:64], in_=dl[:, 0:32], func=AF.Copy, accum_out=lam[:, 0:1]), r=[dl], w=[dl, lam])
        k.op("act", lambda e: e.activation(out=dl[:, 96:128], in_=dl[:, 64:96], func=AF.Copy, accum_out=lam[:, 1:2]), r=[dl], w=[dl, lam])
        k.op("act", lambda e: e.activation(out=lam[:, 0:2], in_=lam[:, 0:2], func=AF.Exp), r=[lam], w=[lam])
        k.op("dve", lambda e: e.tensor_tensor(out=lam[:, 2:3], in0=lam[:, 1:2], in1=lam[:, 0:1], op=ALU.subtract), r=[lam], w=[lam])
        lc = k.sb(es, [64, 2], F32, "lamc")
        k.ld(lc[:], g.lamc[:, :], w=[lc])
        k.op("dve", lambda e: e.tensor_tensor(out=lam[:, 2:3], in0=lam[:, 2:3], in1=lc[:, 0:1], op=ALU.add), r=[lam, lc], w=[lam])
        k.op("dve", lambda e: e.tensor_tensor(out=gd[:], in0=gd[:], in1=lc[:, 1:2], op=ALU.mult), r=[gd, lc], w=[gd])
        neglam = lam

        Kr = k.rot(es, 2, [96, TL], BF16, "Kt")
        Ve = k.sb(es, [128, NKT, 65], BF16, "Ve")
        Vo = k.sb(es, [128, NKT, 65], BF16, "Vo")
        qrr = k.rot(es, 3, [96, 512], BF16, "qr")
        qpr = k.rot(es, 3, [96, 512], BF16, "qp")
        Pr = k.rot(es, 4, [128, 512], BF16, "P")
        osr = k.rot(es, 2, [128, 512], F32, "osb")
        onr = k.rot(es, 3, [64, 512], F32, "on")
        on0r = k.rot(es, 2, [64, 512], F32, "on0")
        dr_ = k.rot(es, 2, [64, 512], F32, "d")
        sqr = k.rot(es, 2, [64, 512], F32, "sq")
        obr = k.rot(es, 3, [64, 512], BF16, "ob")

        def load_v(vt, vcol, off=0, ntile=NKT):
            k.ld(vt[:, 0:ntile, 0:64], g.V[off:off + ntile * 128, vcol:vcol + 64].rearrange("(t p) d -> p t d", p=128), r=[g.V], w=[vt])
            k.op("pool", lambda e: e.memset(vt[:, :, 64:65], 1.0), w=[vt])

        def load_k(krow, dk):
            kt = Kr.get()
            k.ld(kt[0:dk, :], g.KT[krow:krow + dk, :], r=[g.KT], w=[kt])
            return kt

        def store_y(on, n, yrow, c0):
            ob = obr.get()
            k.op("act", lambda e: e.activation(out=ob[0:64, 0:n], in_=on[0:64, 0:n], func=AF.Copy), r=[on], w=[ob])
            k.st(g.YT[yrow:yrow + 64, c0:c0 + n], ob[0:64, 0:n], r=[ob], w=[g.YT])

        def attend(kt, dk, vt, qlat, qctx, n, tiles, scale):
            po = k.pso.get()
            prev = None
            last = len(tiles) - 1

            def pv(p):
                P, t, i = p
                k.mm(po, po[0:65, 0:n], vt[:, t, :], P[:, 0:n], i == 0, i == last, r=[vt, P])
            for i, t in enumerate(tiles):
                ps = k.pss.get()
                q = qlat if t < NLT else qctx
                k.mm(ps, ps[:, 0:n], kt[0:dk, t * 128:(t + 1) * 128], q[0:dk, 0:n], True, True, r=[kt, q])
                if prev is not None:
                    pv(prev)
                P = Pr.get()
                k.op("act", lambda e: e.activation(out=P[:, 0:n], in_=ps[:, 0:n], func=AF.Exp, scale=scale), r=[ps], w=[P])
                prev = (P, t, i)
            pv(prev)
            return po

        def load_q(row, dk, c0, n, both=True):
            qr, qp = qrr.get(), qpr.get()
            k.ld(qr[0:dk, 0:n], g.QR[row:row + dk, c0:c0 + n], r=[g.QR], w=[qr])
            if both:
                k.ld(qp[0:dk, 0:n], g.QP[row:row + dk, c0:c0 + n], r=[g.QP], w=[qp])
            return qr, qp

        chunks = [(c * 512, 512, list(range(NKT))) for c in range(NT // 512)] + [(NT, NCTX, list(range(NLT, NKT)))]

        for h in range(4):
            load_v(Ve, 256 + h * 64)
            kts = [load_k(256 + (2 * h + m) * 32, 32) for m in range(2)]
            sc = 32 ** -0.5
            for (c0, n, tiles) in chunks:
                ons = []
                for m in range(2):
                    row = 256 + (2 * h + m) * 32
                    qr, qp = load_q(row, 32, c0, n)
                    if c0 >= NT:
                        qr = qp
                    po = attend(kts[m], 32, Ve, qr, qp, n, tiles, sc)
                    on = epilogue(k, g, po, n, osr, on0r if m == 0 else onr)
                    ons.append(on)
                d, sq = dr_.get(), sqr.get()
                k.op("dve", lambda e: e.scalar_tensor_tensor(out=d[:, 0:n], in0=ons[1][:, 0:n], scalar=neglam[:, 2:3], in1=ons[0][:, 0:n],
                                                            op0=ALU.mult, op1=ALU.add), r=[ons[0], ons[1], neglam], w=[d])
                k.op("act", lambda e: e.activation(out=sq[:, 0:n], in_=d[:, 0:n], func=AF.Square), r=[d], w=[sq])
                pb = k.pss.get()
                k.mm(pb, pb[0:64, 0:n], g.onesf[0:64, 0:64], sq[0:64, 0:n], True, True, r=[sq, g.onesf])
                k.op("act", lambda e: e.activation(out=sq[:, 0:n], in_=pb[0:64, 0:n], func=AF.Sqrt, scale=1.0 / 64, bias=g.epsT[0:64, :]),
                     r=[pb], w=[sq])
                k.op("dve", lambda e: e.reciprocal(out=sq[:, 0:n], in_=sq[:, 0:n]), r=[sq], w=[sq])
                ob = obr.get()
                k.op("dve", lambda e: e.scalar_tensor_tensor(out=ob[:, 0:n], in0=d[:, 0:n], scalar=gd[:, 0:1], in1=sq[:, 0:n], op0=ALU.mult,
                                                            op1=ALU.mult), r=[d, sq, gd], w=[ob])
                k.st(g.YT[256 + h * 64:256 + h * 64 + 64, c0:c0 + n], ob[0:64, 0:n], r=[ob], w=[g.YT])
        for h in range(4):
            load_v(Ve, 512 + h * 64)
            kt = load_k(512 + h * 96, 96)
            sc = 96 ** -0.5
            for (c0, n, tiles) in chunks:
                qr, qp = load_q(512 + h * 96, 96, c0, n)
                if c0 >= NT:
                    qr = qp
                po = attend(kt, 96, Ve, qr, qp, n, tiles, sc)
                on = epilogue(k, g, po, n, osr, onr)
                store_y(on, n, 512 + h * 64, c0)
        for h in range(4):
            load_v(Ve, h * 64)
            load_v(Vo, h * 64, off=64, ntile=NKT - 1)
            kt = load_k(h * 64, 64)
            sc = 0.125
            for qc in range(NT // 512):
                qr, _ = load_q(h * 64, 64, qc * 512, 512, both=False)
                po = k.pso.get()
                for rl in range(8):
                    r = qc * 8 + rl
                    base = min(max(r - 4, 0), ROWS - 8)
                    o = r - base
                    ps = k.pss.get()
                    qs = qr[0:64, rl * 64:(rl + 1) * 64]
                    for m in range(4):
                        k0 = 64 * (base + 2 * m)
                        k.mm(ps, ps[:, m * 64:(m + 1) * 64], kt[0:64, k0:k0 + 128], qs, True, False, r=[kt, qr])
                        k.mm(ps, ps[:, m * 64:(m + 1) * 64], g.identb[:], nabb[:, h, 2 * m - o + 7, :], False, True, r=[nabb, g.identb])
                    for c in range(2):
                        k.mm(ps, ps[:, (4 + c) * 64:(5 + c) * 64], kt[0:64, NT + c * 128:NT + (c + 1) * 128], qs, True, True, r=[kt, qr])
                    P = Pr.get()
                    k.op("act", lambda e: e.activation(out=P[:, 0:384], in_=ps[:, 0:384], func=AF.Exp, scale=sc), r=[ps], w=[P])
                    for m in range(6):
                        if m < 4:
                            kr = base + 2 * m
                            vt = Ve[:, kr // 2, :] if kr % 2 == 0 else Vo[:, (kr - 1) // 2, :]
                        else:
                            vt = Ve[:, NLT + (m - 4), :]
                        k.mm(po, po[0:65, rl * 64:(rl + 1) * 64], vt, P[:, m * 64:(m + 1) * 64], m == 0, m == 5, r=[Ve, Vo, P])
                on = epilogue(k, g, po, 512, osr, onr)
                store_y(on, 512, h * 64, qc * 512)
            qr, qp = load_q(h * 64, 64, NT, NCTX)
            po = attend(kt, 64, Ve, qp, qp, NCTX, list(range(NLT, NKT)), sc)
            on = epilogue(k, g, po, NCTX, osr, onr)
            store_y(on, NCTX, h * 64, NT)
        k.tk.barrier()


def phase_s5(k, l, g):
    NT, TL, NLT = k.NT, k.TL, k.NLT
    NCH, NL, NCX = TL // 8, NT // 8, NCTX // 8
    GB = 2
    nsteps = int(math.ceil(math.log2(NCH)))
    k.pss = Rot(k.psr.tiles[0:8])
    TT = lambda e, o, a, b_, op: e.tensor_tensor(out=o, in0=a, in1=b_, op=op)
    with ExitStack() as es:
        msk = k.sb(es, [128, 2, 128], F32, "msk")
        Jm = k.sb(es, [128, 128], F32, "J")
        sg = k.sb(es, [128, 2], F32, "sg")
        kv = k.sb(es, [128, 48], F32, "kv")
        k.ld(msk[:], g.s5msk.rearrange("d p c -> p d c"), w=[msk])
        k.ld(Jm[:], g.s5J, w=[Jm])
        k.ld(sg[:], g.s5sg, w=[sg])
        k.ld(kv[:], g.s5kv, w=[kv])
        Mm = [k.sb(es, [128, 16, 128], F32, f"M{d}") for d in range(2)]
        BcT = [k.sb(es, [128, 16, 128], F32, f"BcT{d}") for d in range(2)]
        Cc = [k.sb(es, [128, 16, 128], F32, f"Cc{d}") for d in range(2)]
        PWI = [k.sb(es, [128, nsteps, 16], F32, f"PWI{d}") for d in range(2)]
        PWJ = [k.sb(es, [128, nsteps, 16], F32, f"PWJ{d}") for d in range(2)]
        with ExitStack() as s2:
            def tmp(shape, nm):
                return k.sb(s2, shape, F32, nm)
            for d in range(2):
                p = tmp([128, 48], "p")
                k.ld(p[:], g.s5p[l, d], w=[p])
                BA, BB = tmp([128, 16, 16], "BA"), tmp([128, 16, 16], "BB")
                CA, CB = tmp([128, 16, 16], "CA"), tmp([128, 16, 16], "CB")
                k.ld(BA[:].rearrange("p a b -> p (a b)"), g.s5B[l, d, 0], w=[BA])
                k.ld(BB[:].rearrange("p a b -> p (a b)"), g.s5B[l, d, 1], w=[BB])
                k.ld(CA[:].rearrange("p a b -> p (a b)"), g.s5C[l, d, 0], w=[CA])
                k.ld(CB[:].rearrange("p a b -> p (a b)"), g.s5C[l, d, 1], w=[CB])
                k.op("dve", lambda e: e.tensor_scalar(out=BB[:], in0=BB[:], scalar1=sg[:, 0:1], scalar2=None, op0=ALU.mult), r=[BB, sg], w=[BB])
                k.op("dve", lambda e: e.tensor_scalar(out=CA[:], in0=CA[:], scalar1=sg[:, 1:2], scalar2=None, op0=ALU.mult), r=[CA, sg], w=[CA])
                k.op("dve", lambda e: e.tensor_scalar(out=CB[:], in0=CB[:], scalar1=-1.0, scalar2=None, op0=ALU.mult), r=[CB], w=[CB])
                st_ = tmp([128, 16], "step")
                lrs, lis = tmp([128, 16], "lrs"), tmp([128, 16], "lis")
                k.op("act", lambda e: e.activation(out=st_[:], in_=p[:, 32:48], func=AF.Exp), r=[p], w=[st_])
                k.op("dve", lambda e: TT(e, lrs[:], p[:, 0:16], st_[:], ALU.mult), r=[p, st_], w=[lrs])
                k.op("dve", lambda e: TT(e, lis[:], p[:, 16:32], st_[:], ALU.mult), r=[p, st_], w=[lis])
                PR, PI = {}, {}
                for u in (range(0, 3) if d == 0 else range(3, 6)):
                    kvu = kv[:, u * 8:(u + 1) * 8].unsqueeze(1).broadcast_to([128, 16, 8])
                    mag, ang, ti, tf = tmp([128, 16, 8], "mag"), tmp([128, 16, 8], "ang"), k.sb(s2, [128, 16, 8], I32, "ti"), tmp([128, 16, 8], "tf")
                    pr, pi = tmp([128, 16, 8], "pr"), tmp([128, 16, 8], "pi")
                    k.op("dve", lambda e: TT(e, mag[:], lrs[:].unsqueeze(2).broadcast_to([128, 16, 8]), kvu, ALU.mult), r=[lrs, kv], w=[mag])
                    k.op("act", lambda e: e.activation(out=mag[:], in_=mag[:], func=AF.Exp), r=[mag], w=[mag])
                    k.op("dve", lambda e: TT(e, ang[:], lis[:].unsqueeze(2).broadcast_to([128, 16, 8]), kvu, ALU.mult), r=[lis, kv], w=[ang])
                    for (dst, off) in ((pi, 0.0), (pr, 0.25)):
                        k.op("dve", lambda e: e.tensor_scalar(out=tf[:], in0=ang[:], scalar1=1.0 / TWO_PI, scalar2=off, op0=ALU.mult, op1=ALU.add),
                             r=[ang], w=[tf])
                        k.op("dve", lambda e: e.tensor_copy(out=ti[:], in_=tf[:]), r=[tf], w=[ti])
                        k.op("dve", lambda e: e.tensor_copy(out=dst[:], in_=ti[:]), r=[ti], w=[dst])
                        k.op("dve", lambda e: TT(e, tf[:], tf[:], dst[:], ALU.subtract), r=[tf, dst], w=[tf])
                        k.op("act", lambda e: e.activation(out=dst[:], in_=tf[:], func=AF.Sin, scale=TWO_PI * (1.0 - 1e-6)), r=[tf], w=[dst])
                        k.op("dve", lambda e: TT(e, dst[:], dst[:], mag[:], ALU.mult), r=[dst, mag], w=[dst])
                    PR[u], PI[u] = pr, pi
                uB, uC, uM = (0, 1, 2) if d == 0 else (3, 4, 5)
                i1, i8 = (0, 7) if d == 0 else (7, 0)
                ar1, den, fr, fi, t0 = tmp([128, 16], "ar1"), tmp([128, 16], "den"), tmp([128, 16], "fr"), tmp([128, 16], "fi"), tmp([128, 16], "t0")
                ai = PI[uC][:, :, i1]
                lr_, li_ = p[:, 0:16], p[:, 16:32]
                k.op("dve", lambda e: e.tensor_scalar(out=ar1[:], in0=PR[uC][:, :, i1], scalar1=-1.0, scalar2=None, op0=ALU.add), r=[PR[uC]], w=[ar1])
                k.op("dve", lambda e: TT(e, den[:], lr_, lr_, ALU.mult), r=[p], w=[den])
                k.op("dve", lambda e: TT(e, t0[:], li_, li_, ALU.mult), r=[p], w=[t0])
                k.op("dve", lambda e: TT(e, den[:], den[:], t0[:], ALU.add), r=[den, t0], w=[den])
                k.op("dve", lambda e: e.reciprocal(out=den[:], in_=den[:]), r=[den], w=[den])
                k.op("dve", lambda e: TT(e, fr[:], ar1[:], lr_, ALU.mult), r=[ar1, p], w=[fr])
                k.op("dve", lambda e: TT(e, t0[:], ai, li_, ALU.mult), r=[PI[uC], p], w=[t0])
                k.op("dve", lambda e: TT(e, fr[:], fr[:], t0[:], ALU.add), r=[fr, t0], w=[fr])
                k.op("dve", lambda e: TT(e, fr[:], fr[:], den[:], ALU.mult), r=[fr, den], w=[fr])
                k.op("dve", lambda e: TT(e, fi[:], ai, lr_, ALU.mult), r=[PI[uC], p], w=[fi])
                k.op("dve", lambda e: TT(e, t0[:], ar1[:], li_, ALU.mult), r=[ar1, p], w=[t0])
                k.op("dve", lambda e: TT(e, fi[:], fi[:], t0[:], ALU.subtract), r=[fi, t0], w=[fi])
                k.op("dve", lambda e: TT(e, fi[:], fi[:], den[:], ALU.mult), r=[fi, den], w=[fi])
                gr, gi, t8 = tmp([128, 16, 8], "gr"), tmp([128, 16, 8], "gi"), tmp([128, 16, 8], "t8")
                frb = fr[:].unsqueeze(2).broadcast_to([128, 16, 8])
                fib = fi[:].unsqueeze(2).broadcast_to([128, 16, 8])
                k.op("dve", lambda e: TT(e, gr[:], PR[uB][:], frb, ALU.mult), r=[PR[uB], fr], w=[gr])
                k.op("dve", lambda e: TT(e, t8[:], PI[uB][:], fib, ALU.mult), r=[PI[uB], fi], w=[t8])
                k.op("dve", lambda e: TT(e, gr[:], gr[:], t8[:], ALU.subtract), r=[gr, t8], w=[gr])
                k.op("dve", lambda e: TT(e, gi[:], PR[uB][:], fib, ALU.mult), r=[PR[uB], fi], w=[gi])
                k.op("dve", lambda e: TT(e, t8[:], PI[uB][:], frb, ALU.mult), r=[PI[uB], fr], w=[t8])
                k.op("dve", lambda e: TT(e, gi[:], gi[:], t8[:], ALU.add), r=[gi, t8], w=[gi])

                def outer(dst, XA, XB, sr, si):
                    dv = dst[:].rearrange("p g (s i) -> p g s i", i=16)
                    t4 = tmp([128, 16, 8, 16], "t4")
                    bc = lambda x: x[:].unsqueeze(2).broadcast_to([128, 16, 8, 16])
                    sc_ = lambda x: x[:].unsqueeze(3).broadcast_to([128, 16, 8, 16])
                    k.op("dve", lambda e: TT(e, dv, bc(XA), sc_(sr), ALU.mult), r=[XA, sr], w=[dst])
                    k.op("dve", lambda e: TT(e, t4[:], bc(XB), sc_(si), ALU.mult), r=[XB, si], w=[t4])
                    k.op("dve", lambda e: TT(e, dv, dv, t4[:], ALU.add), r=[dst, t4], w=[dst])
                Bc, Cm = tmp([128, 16, 128], "Bc"), tmp([128, 16, 128], "Cm")
                outer(Bc, BA, BB, gr, gi)
                outer(Cc[d], CA, CB, PR[uC], PI[uC])
                outer(Cm, CA, CB, PR[uM], PI[uM])
                for gg in range(16):
                    ps = k.pss.get()
                    k.mm(ps, ps[:, 0:128], Bc[:, gg, :], Cm[:, gg, :], True, True, r=[Bc, Cm])
                    k.op("dve", lambda e: TT(e, Mm[d][:, gg, :], ps[:, 0:128], msk[:, d, :], ALU.mult), r=[ps, msk], w=[Mm[d]])
                    ps = k.pss.get()
                    k.op("pe", lambda e: e.transpose(ps[:, 0:128], Bc[:, gg, :], g.identf[:]), r=[Bc, g.identf], w=[ps])
                    k.op("act", lambda e: e.activation(out=BcT[d][:, gg, :], in_=ps[:, 0:128], func=AF.Copy), r=[ps], w=[BcT[d]])
                k.op("dve", lambda e: e.tensor_copy(out=PWI[d][:, 0, :], in_=PR[uC][:, :, i8]), r=[PR[uC]], w=[PWI[d]])
                k.op("dve", lambda e: e.tensor_scalar(out=PWJ[d][:, 0, :], in0=PI[uC][:, :, i8], scalar1=sg[:, 1:2], scalar2=None, op0=ALU.mult),
                     r=[PI[uC], sg], w=[PWJ[d]])
                for m in range(1, nsteps):
                    a_, b_ = PWI[d][:, m - 1, :], PWJ[d][:, m - 1, :]
                    k.op("dve", lambda e: TT(e, t0[:], b_, b_, ALU.mult), r=[PWJ[d]], w=[t0])
                    k.op("dve", lambda e: TT(e, PWI[d][:, m, :], a_, a_, ALU.mult), r=[PWI[d]], w=[PWI[d]])
                    k.op("dve", lambda e: TT(e, PWI[d][:, m, :], PWI[d][:, m, :], t0[:], ALU.subtract), r=[PWI[d], t0], w=[PWI[d]])
                    k.op("dve", lambda e: TT(e, PWJ[d][:, m, :], a_, b_, ALU.mult), r=[PWI[d], PWJ[d]], w=[PWJ[d]])
                    k.op("dve", lambda e: e.tensor_scalar(out=PWJ[d][:, m, :], in0=PWJ[d][:, m, :], scalar1=2.0, scalar2=None, op0=ALU.mult),
                         r=[PWJ[d]], w=[PWJ[d]])
            k.tk.barrier()
        U = k.sb(es, [128, GB, NCH], F32, "U")
        HP = k.sb(es, [128, GB, NCH + 2], F32, "HP")
        Ya = k.sb(es, [128, GB, NCH], F32, "Ya")
        ucr = k.rot(es, 2, [128, 2048], F32, "uc")
        ugr = k.rot(es, 3, [128, 128], F32, "ug")
        Ar = k.rot(es, 3, [128, 128], F32, "Arot")
        ycr = k.rot(es, 2, [128, 8, GB, 16], F32, "yc")
        UTv = g.UT.t.rearrange("(c s) f -> c (s f)", s=8)
        YSv = g.YS.t.rearrange("(c s) (gg j) -> c s gg j", s=8, j=16)
        ctiles = [(t * 128, 128) for t in range(NL // 128)] + [(NL, NCX)]
        for gb in range(16 // GB):
            for (c0, rows) in ctiles:
                uc = ucr.get()
                k.ld(uc[0:rows, :], UTv[c0:c0 + rows, :], r=[g.UT], w=[uc])
                ucv = uc[:].rearrange("p (s gg i) -> p s gg i", s=8, i=16)
                for gi_ in range(GB):
                    ps = k.pss.get()
                    ug = ugr.get()
                    k.op("pool", lambda e: e.tensor_copy(out=ug[0:rows, :].rearrange("p (s i) -> p s i", i=16), in_=ucv[0:rows, :, gb * GB + gi_, :]),
                         r=[uc], w=[ug])
                    k.op("pe", lambda e: e.transpose(ps[:, 0:rows], ug[0:rows, :], g.identf[0:rows, 0:rows]), r=[ug, g.identf], w=[ps])
                    k.op("act", lambda e: e.activation(out=U[:, gi_, c0:c0 + rows], in_=ps[:, 0:rows], func=AF.Copy), r=[ps], w=[U])
            for d in range(2):
                k.op("pool", lambda e: e.memset(HP[:], 0.0), w=[HP])
                if d == 0:
                    pieces = [(0, NL, NCX)] + [(NCX + a, a, min(512, NL - a)) for a in range(0, NL, 512)]
                else:
                    pieces = [(a, a, min(512, NCH - a)) for a in range(0, NCH, 512)]
                for gi_ in range(GB):
                    gg = gb * GB + gi_
                    for (p0, n0, n) in pieces:
                        ps = k.pss.get()
                        k.mm(ps, ps[:, 0:n], BcT[d][:, gg, :], U[:, gi_, n0:n0 + n], True, True, r=[BcT[d], U])
                        k.op("act", lambda e: e.activation(out=HP[:, gi_, 1 + p0:1 + p0 + n], in_=ps[:, 0:n], func=AF.Copy), r=[ps], w=[HP])
                    for m in range(nsteps):
                        kk = 1 << m
                        if kk >= NCH:
                            break
                        A, T1 = Ar.get(), Ar.get()
                        k.op("dve", lambda e: e.tensor_scalar(out=T1[:], in0=Jm[:], scalar1=PWJ[d][:, m, gg:gg + 1], scalar2=None, op0=ALU.mult),
                             r=[Jm, PWJ[d]], w=[T1])
                        k.op("dve", lambda e: e.scalar_tensor_tensor(out=A[:], in0=g.identf[:], scalar=PWI[d][:, m, gg:gg + 1], in1=T1[:],
                                                                    op0=ALU.mult, op1=ALU.add), r=[g.identf, PWI[d], T1], w=[A])
                        nn = NCH - kk
                        starts = list(range(0, nn, 512))
                        if d == 0:
                            starts = starts[::-1]
                        for s0 in starts:
                            n = min(512, nn - s0)
                            ps = k.pss.get()
                            if d == 0:
                                src, dst = HP[:, gi_, 1 + s0:1 + s0 + n], HP[:, gi_, 1 + kk + s0:1 + kk + s0 + n]
                            else:
                                src, dst = HP[:, gi_, 1 + kk + s0:1 + kk + s0 + n], HP[:, gi_, 1 + s0:1 + s0 + n]
                            k.mm(ps, ps[:, 0:n], A[:], src, True, True, r=[A, HP])
                            k.op("dve", lambda e: TT(e, dst, dst, ps[:, 0:n], ALU.add), r=[HP, ps], w=[HP])
                    for (p0, n0, n) in pieces:
                        ps = k.pss.get()
                        sh = p0 if d == 0 else p0 + 2
                        k.mm(ps, ps[:, 0:n], Mm[d][:, gg, :], U[:, gi_, n0:n0 + n], True, False, r=[Mm[d], U])
                        k.mm(ps, ps[:, 0:n], Cc[d][:, gg, :], HP[:, gi_, sh:sh + n], False, True, r=[Cc[d], HP])
                        if d == 0:
                            k.op("act", lambda e: e.activation(out=Ya[:, gi_, n0:n0 + n], in_=ps[:, 0:n], func=AF.Copy), r=[ps], w=[Ya])
                        else:
                            k.op("dve", lambda e: TT(e, Ya[:, gi_, n0:n0 + n], Ya[:, gi_, n0:n0 + n], ps[:, 0:n], ALU.add), r=[Ya, ps], w=[Ya])
            for (c0, rows) in ctiles:
                yc = ycr.get()
                for gi_ in range(GB):
                    ps = k.pss.get()
                    k.op("pe", lambda e: e.transpose(ps[0:rows, 0:128], Ya[:, gi_, c0:c0 + rows], g.identf[:]), r=[Ya, g.identf], w=[ps])
                    k.op("act", lambda e: e.activation(out=yc[0:rows, :, gi_, :], in_=ps[0:rows, 0:128].rearrange("p (s j) -> p s j", j=16),
                                                      func=AF.Copy), r=[ps], w=[yc])
                k.st(YSv[c0:c0 + rows, :, gb * GB:(gb + 1) * GB, :], yc[0:rows], r=[yc], w=[g.YS])
        k.tk.barrier()
    with ExitStack() as es:
        k.stage = k.rot(es, 1, [128, 512], F32, "stage")
        wg = k.sb(es, [128, 2, 256], BF16, "wglu")
        k.load_cast(es, wg, wg[:], g.w_glu[l].rearrange("(kc p) c -> p kc c", p=128), [128, 2, 256])
        Dt = k.sb(es, [128, 256], F32, "Dt")
        bg = k.sb(es, [128, 256], F32, "bg")
        k.ld(Dt[:], g.s5_d[l:l + 1, :].partition_broadcast(128), w=[Dt])
        k.ld(bg[:], g.b_glu[l:l + 1, :].partition_broadcast(128), w=[bg])
        yr, ur, t1r, t2r = (k.rot(es, 2, [128, 256], F32, nm) for nm in ("y", "u", "t1", "t2"))
        gbr = k.rot(es, 2, [128, 256], BF16, "gb")
        gtr = k.rot(es, 2, [128, 2, 128], BF16, "gt")
        C1 = 2.0 * math.sqrt(2.0 / math.pi)
        for t in range(k.NTI):
            cs = slice(t * 128, (t + 1) * 128)
            y, u, t1, t2, gb_, gt = yr.get(), ur.get(), t1r.get(), t2r.get(), gbr.get(), gtr.get()
            k.ld(y[:], g.YS[cs, :], r=[g.YS], w=[y])
            k.ld(u[:], g.UT[cs, :], r=[g.UT], w=[u])
            k.op("dve", lambda e: TT(e, u[:], u[:], Dt[:], ALU.mult), r=[u, Dt], w=[u])
            k.op("dve", lambda e: TT(e, y[:], y[:], u[:], ALU.add), r=[y, u], w=[y])
            k.op("act", lambda e: e.activation(out=t1[:], in_=y[:], func=AF.Square), r=[y], w=[t1])
            k.op("dve", lambda e: e.tensor_scalar(out=t1[:], in0=t1[:], scalar1=0.044715, scalar2=1.0, op0=ALU.mult, op1=ALU.add), r=[t1], w=[t1])
            k.op("dve", lambda e: TT(e, t1[:], t1[:], y[:], ALU.mult), r=[t1, y], w=[t1])
            k.op("act", lambda e: e.activation(out=t1[:], in_=t1[:], func=AF.Sigmoid, scale=C1), r=[t1], w=[t1])
            k.op("dve", lambda e: TT(e, y[:], y[:], t1[:], ALU.mult), r=[y, t1], w=[y])
            k.op("act", lambda e: e.activation(out=gb_[:], in_=y[:], func=AF.Copy), r=[y], w=[gb_])
            pt = k.pss.get()
            ptv = pt[:].bitcast(BF16)
            for i in range(2):
                k.op("pe", lambda e: e.transpose(ptv[:, i * 128:(i + 1) * 128], gb_[:, i * 128:(i + 1) * 128], g.identb[:]), r=[gb_, g.identb], w=[pt])
            k.op("act", lambda e: e.activation(out=gt[:].rearrange("p a b -> p (a b)"), in_=ptv[:, 0:256], func=AF.Copy), r=[pt], w=[gt])
            ps = k.pss.get()
            for kc in range(2):
                k.mm(ps, ps[:, 0:256], gt[:, kc, :], wg[:, kc, :], kc == 0, kc == 1, r=[gt, wg])
            k.op("dve", lambda e: TT(e, t2[:], ps[:, 0:256], bg[:], ALU.add), r=[ps, bg], w=[t2])
            k.op("act", lambda e: e.activation(out=t2[:], in_=t2[:], func=AF.Sigmoid), r=[t2], w=[t2])
            k.op("dve", lambda e: TT(e, gb_[:], y[:], t2[:], ALU.mult), r=[y, t2], w=[gb_])
            pt = k.pss.get()
            ptv = pt[:].bitcast(BF16)
            for i in range(2):
                k.op("pe", lambda e: e.transpose(ptv[:, i * 128:(i + 1) * 128], gb_[:, i * 128:(i + 1) * 128], g.identb[:]), r=[gb_, g.identb], w=[pt])
            gt2 = gtr.get()
            k.op("act", lambda e: e.activation(out=gt2[:].rearrange("p a b -> p (a b)"), in_=ptv[:, 0:256], func=AF.Copy), r=[pt], w=[gt2])
            k.st(g.YT[768:1024, cs].rearrange("(kc p) t -> p kc t", p=128), gt2[:], r=[gt2], w=[g.YT])
        k.tk.barrier()


def phase_merge(k, l, g):
    NT, TL = k.NT, k.TL
    TT = lambda e, o, a, b_, op: e.tensor_tensor(out=o, in0=a, in1=b_, op=op)
    k.pss = Rot(k.psr.tiles[0:8])
    with ExitStack() as es:
        k.stage = k.rot(es, 1, [128, 4096], F32, "stage")
        wg = k.sb(es, [128, 8, 4096], BF16, "wgate")
        for kc in range(8):
            k.load_cast(es, wg, wg[:, kc, :], g.w_in[l, kc * 128:(kc + 1) * 128, 2208:INW], [128, 4096], eng="dve" if kc % 2 else "pool")
        wb = k.sb(es, [128, 4, 2, D], BF16, "wbr")
        for i in range(4):
            k.load_cast(es, wb, wb[:, i, :, :], g.w_branch[l, i].rearrange("(kc p) c -> p kc c", p=128), [128, 2, D])
        wo = k.sb(es, [128, 8, D], BF16, "wout")
        for h2 in range(2):
            k.load_cast(es, wo, wo[:, h2 * 4:(h2 + 1) * 4, :], g.w_out[l, h2 * 512:(h2 + 1) * 512, :].rearrange("(kc p) c -> p kc c", p=128), [128, 4, D])
        wr = k.sb(es, [128, 8, 64], BF16, "wrt")
        k.load_cast(es, wr, wr[:], g.w_router[l].rearrange("(kc p) c -> p kc c", p=128), [128, 8, 64])
        eb = k.sb(es, [128, 64], F32, "eb")
        k.ld(eb[:], g.e_bias[l:l + 1, :].partition_broadcast(128), w=[eb])
        hr = k.rot(es, 2, [128, 8, 128], BF16, "hT")
        yr_ = k.rot(es, 2, [128, 8, 128], BF16, "yT")
        xr = k.rot(es, 2, [128, D], F32, "x")
        sgr = k.rot(es, 2, [128, 512], F32, "sig")
        mr = k.rot(es, 2, [128, D], F32, "m")
        mbr = k.rot(es, 2, [128, D], BF16, "mb")
        mtr = k.rot(es, 2, [128, 8, 128], BF16, "mT")
        jr = k.rot(es, 1, [128, D], F32, "j")
        sr = k.rot(es, 2, [128, 1], F32, "ss")
        xnr = k.rot(es, 2, [128, D], BF16, "xn")
        h2r = k.rot(es, 2, [128, 8, 128], BF16, "h2T")
        rtr = k.rot(es, 2, [128, 4, 64], F32, "rt")
        gmr = k.rot(es, 2, [128, 65], F32, "gm")
        t8r = k.rot(es, 2, [128, 8], F32, "t8")
        for t in range(k.NTI):
            b = 0 if t < k.NLT else 1
            cs = slice(t * 128, (t + 1) * 128)
            hT, yT, xt, m = hr.get(), yr_.get(), xr.get(), mr.get()
            k.ld(hT[:], g.HT[:, cs].rearrange("(kc p) t -> p kc t", p=128), r=[g.HT], w=[hT])
            k.ld(yT[:], g.YT[:, cs].rearrange("(kc p) t -> p kc t", p=128), r=[g.YT], w=[yT])
            k.ld(xt[:], g.X[cs, :], r=[g.X], w=[xt])
            for i in range(4):
                for hf in range(2):
                    pg, pbm = k.pss.get(), k.pss.get()
                    for kc in range(8):
                        k.mm(pg, pg[:, :], hT[:, kc, :], wg[:, kc, i * 1024 + hf * 512:i * 1024 + (hf + 1) * 512], kc == 0, kc == 7, r=[hT, wg])
                    for kc in range(2):
                        k.mm(pbm, pbm[:, :], yT[:, 2 * i + kc, :], wb[:, i, kc, hf * 512:(hf + 1) * 512], kc == 0, kc == 1, r=[yT, wb])
                    sg_ = sgr.get()
                    k.op("act", lambda e: e.activation(out=sg_[:], in_=pg[:, :], func=AF.Sigmoid), r=[pg], w=[sg_])
                    ms = m[:, hf * 512:(hf + 1) * 512]
                    if i == 0:
                        k.op("dve", lambda e: TT(e, ms, sg_[:], pbm[:, :], ALU.mult), r=[sg_, pbm], w=[m])
                    else:
                        k.op("dve", lambda e: TT(e, sg_[:], sg_[:], pbm[:, :], ALU.mult), r=[sg_, pbm], w=[sg_])
                        k.op("pool", lambda e: TT(e, ms, ms, sg_[:], ALU.add), r=[sg_, m], w=[m])
            mb_, mT = mbr.get(), mtr.get()
            k.op("act", lambda e: e.activation(out=mb_[:], in_=m[:], func=AF.Copy), r=[m], w=[mb_])
            pt = k.pss.get()
            ptv = pt[:].bitcast(BF16)
            for kc in range(8):
                k.op("pe", lambda e: e.transpose(ptv[:, kc * 128:(kc + 1) * 128], mb_[:, kc * 128:(kc + 1) * 128], g.identb[:]), r=[mb_, g.identb], w=[pt])
            k.op("act", lambda e: e.activation(out=mT[:].rearrange("p a b -> p (a b)"), in_=ptv[:, :], func=AF.Copy), r=[pt], w=[mT])
            for hf in range(2):
                py = k.pss.get()
                for kc in range(8):
                    k.mm(py, py[:, :], mT[:, kc, :], wo[:, kc, hf * 512:(hf + 1) * 512], kc == 0, kc == 7, r=[mT, wo])
                sg_ = sgr.get()
                xs = xt[:, hf * 512:(hf + 1) * 512]
                k.op("dve", lambda e: TT(e, sg_[:], py[:, :], k.GT[0][b][:, hf * 512:(hf + 1) * 512], ALU.mult), r=[py, k.GT[0][b]], w=[sg_])
                k.op("pool", lambda e: TT(e, xs, xs, sg_[:], ALU.add), r=[sg_, xt], w=[xt])
            k.st(g.X[cs, :], xt[:], r=[xt], w=[g.X])
            h2T = h2r.get()
            norm_mod_T(k, (jr.get(), sr.get(), xnr.get()), xt, k.A2, k.B2, b, g.epsT, g.identb, h2T)
            k.st(g.H2T[:, cs].rearrange("(kc p) t -> p kc t", p=128), h2T[:], r=[h2T], w=[g.H2T])
            pl_ = k.pss.get()
            for kc in range(8):
                k.mm(pl_, pl_[:, 0:64], h2T[:, kc, :], wr[:, kc, :], kc == 0, kc == 7, r=[h2T, wr])
            rt, gm, t8 = rtr.get(), gmr.get(), t8r.get()
            sco, sel, msk_ = rt[:, 0, :], rt[:, 1, :], rt[:, 2, :]
            k.op("act", lambda e: e.activation(out=sco, in_=pl_[:, 0:64], func=AF.Sigmoid), r=[pl_], w=[rt])
            k.op("dve", lambda e: TT(e, sel, sco, eb[:], ALU.add), r=[rt, eb], w=[rt])
            k.op("dve", lambda e: e.max(out=t8[:], in_=sel), r=[rt], w=[t8])
            k.op("dve", lambda e: e.tensor_scalar(out=msk_, in0=sel, scalar1=t8[:, 5:6], scalar2=None, op0=ALU.is_ge), r=[rt, t8], w=[rt])
            k.op("dve", lambda e: TT(e, msk_, msk_, sco, ALU.mult), r=[rt], w=[rt])
            k.op("act", lambda e: e.activation(out=rt[:, 3, :], in_=msk_, func=AF.Copy, accum_out=t8[:, 6:7]), r=[rt], w=[rt, t8])
            k.op("dve", lambda e: e.reciprocal(out=t8[:, 7:8], in_=t8[:, 6:7]), r=[t8], w=[t8])
            k.op("dve", lambda e: e.tensor_scalar(out=gm[:, 0:64], in0=msk_, scalar1=t8[:, 7:8], scalar2=None, op0=ALU.mult), r=[rt, t8], w=[gm])
            k.op("pool", lambda e: e.memset(gm[:, 64:65], 1.0), w=[gm])
            k.st(g.GM[cs, :], gm[:], r=[gm], w=[g.GM])


def phase_moe(k, l, g):
    NT, TL = k.NT, k.TL
    TT = lambda e, o, a, b_, op: e.tensor_tensor(out=o, in0=a, in1=b_, op=op)
    k.pss = Rot(k.psr.tiles[0:8])
    with ExitStack() as es:
        with ExitStack() as s2:
            f1 = k.rot(s2, 2, [128, 8, 256], F32, "f1")
            f3 = k.rot(s2, 2, [128, 8, 256], F32, "f3")
            f2 = k.rot(s2, 2, [128, 2, D], F32, "f2")
            bo = k.rot(s2, 2, [128, 6144], BF16, "bo")
            for e_ in range(65):
                a1, a3, a2, o = f1.get(), f3.get(), f2.get(), bo.get()
                s1, s3, s2_ = (g.w_e1[l, e_], g.w_e3[l, e_], g.w_e2[l, e_]) if e_ < 64 else (g.w_s1[l], g.w_s3[l], g.w_s2[l])
                k.ld(a1[:], s1.rearrange("(kc p) c -> p kc c", p=128), w=[a1])
                k.ld(a3[:], s3.rearrange("(kc p) c -> p kc c", p=128), w=[a3])
                k.ld(a2[:], s2_.rearrange("(kc p) c -> p kc c", p=128), w=[a2])
                k.op("dve", lambda e: e.tensor_copy(out=o[:, 0:2048], in_=a1[:].rearrange("p a b -> p (a b)")), r=[a1], w=[o])
                k.op("pool", lambda e: e.tensor_copy(out=o[:, 2048:4096], in_=a3[:].rearrange("p a b -> p (a b)")), r=[a3], w=[o])
                k.op("act", lambda e: e.activation(out=o[:, 4096:6144], in_=a2[:].rearrange("p a b -> p (a b)"), func=AF.Copy), r=[a2], w=[o])
                k.st(g.EW[e_], o[:], r=[o], w=[g.EW])
            k.tk.barrier()
        ewr = k.rot(es, 3, [128, 6144], BF16, "ew")
        h2r = k.rot(es, 2, [128, 8, 512], BF16, "h2c")
        gmr = k.rot(es, 2, [128, 4, 65], F32, "gmc")
        accr = k.rot(es, 2, [128, 4, D], F32, "acc")
        s1r = k.rot(es, 2, [128, 512], F32, "s1")
        ttr = k.rot(es, 2, [128, 2, 512], BF16, "tT")
        xr = k.rot(es, 2, [128, D], F32, "x")
        chunks = [(c * 512, 512) for c in range(NT // 512)] + [(NT, NCTX)]
        for (c0, n) in chunks:
            b = 0 if c0 < NT else 1
            ntt = n // 128
            h2, gm, acc = h2r.get(), gmr.get(), accr.get()
            k.ld(h2[:, :, 0:n], g.H2T[:, c0:c0 + n].rearrange("(kc p) t -> p kc t", p=128), r=[g.H2T], w=[h2])
            k.ld(gm[:, 0:ntt, :], g.GM[c0:c0 + n, :].rearrange("(tt p) e -> p tt e", p=128), r=[g.GM], w=[gm])
            for e_ in range(65):
                ew = ewr.get()
                k.ld(ew[:], g.EW[e_], r=[g.EW], w=[ew])
                tT = ttr.get()
                for hf in range(2):
                    p1, p3 = k.pss.get(), k.pss.get()
                    for kc in range(8):
                        k.mm(p1, p1[:, 0:n], ew[:, kc * 256 + hf * 128:kc * 256 + (hf + 1) * 128], h2[:, kc, 0:n], kc == 0, kc == 7, r=[ew, h2])
                    for kc in range(8):
                        k.mm(p3, p3[:, 0:n], ew[:, 2048 + kc * 256 + hf * 128:2048 + kc * 256 + (hf + 1) * 128], h2[:, kc, 0:n], kc == 0, kc == 7, r=[ew, h2])
                    s1 = s1r.get()
                    k.op("act", lambda e: e.activation(out=s1[:, 0:n], in_=p1[:, 0:n], func=AF.Silu), r=[p1], w=[s1])
                    k.op("dve", lambda e: TT(e, tT[:, hf, 0:n], s1[:, 0:n], p3[:, 0:n], ALU.mult), r=[s1, p3], w=[tT])
                for tt in range(ntt):
                    for nh in range(2):
                        py = k.pss.get()
                        for hf in range(2):
                            k.mm(py, py[:, :], tT[:, hf, tt * 128:(tt + 1) * 128], ew[:, 4096 + hf * 1024 + nh * 512:4096 + hf * 1024 + (nh + 1) * 512],
                                 hf == 0, hf == 1, r=[tT, ew])
                        av = acc[:, tt, nh * 512:(nh + 1) * 512]
                        if e_ == 0:
                            k.op("dve", lambda e: e.tensor_scalar(out=av, in0=py[:, :], scalar1=gm[:, tt, e_:e_ + 1], scalar2=None, op0=ALU.mult),
                                 r=[py, gm], w=[acc])
                        else:
                            k.op("dve", lambda e: e.scalar_tensor_tensor(out=av, in0=py[:, :], scalar=gm[:, tt, e_:e_ + 1], in1=av, op0=ALU.mult,
                                                                        op1=ALU.add), r=[py, gm, acc], w=[acc])
            for tt in range(ntt):
                xt = xr.get()
                cs = slice(c0 + tt * 128, c0 + (tt + 1) * 128)
                k.ld(xt[:], g.X[cs, :], r=[g.X], w=[xt])
                k.op("pool", lambda e: TT(e, acc[:, tt, :], acc[:, tt, :], k.GT[1][b][:], ALU.mult), r=[acc, k.GT[1][b]], w=[acc])
                k.op("pool", lambda e: TT(e, xt[:], xt[:], acc[:, tt, :], ALU.add), r=[acc, xt], w=[xt])
                k.st(g.X[cs, :], xt[:], r=[xt], w=[g.X])
        k.tk.barrier()
    k.lay.close()


def _rope_tables(SEQ):
    TL = SEQ + NCTX
    pos = np.arange(SEQ)
    rows, cols = pos // GW, pos % GW
    freqs = (10000.0 ** (-np.arange(8, dtype=np.float32) * 2.0 / 16)).astype(np.float32)
    C = np.ones((32, TL), np.float32)
    S = np.zeros((32, TL), np.float32)
    for base, p in ((0, rows), (16, cols)):
        ang = p.astype(np.float32)[None, :] * freqs[:, None]
        c, s = np.cos(ang), np.sin(ang)
        C[base:base + 8, :SEQ] = c
        C[base + 8:base + 16, :SEQ] = c
        S[base:base + 8, :SEQ] = -s
        S[base + 8:base + 16, :SEQ] = s
    return np.tile(C, (4, 1)), np.tile(S, (4, 1))


def _na_bias(rpb):
    L = rpb.shape[0]
    col = np.arange(GW)
    cstart = np.clip(col - 8, 0, GW - 16)
    valid = (col[None, :] >= cstart[:, None]) & (col[None, :] < cstart[:, None] + 16)
    dc = np.clip(col[None, :] - col[:, None], -15, 15) + 15
    out = np.empty((L, 2, GW, 4, 14, GW), np.float32)
    for jj in range(2):
        for d in range(14):
            v = rpb[:, :, d + jj, :][:, :, dc]
            v = np.where(valid[None, None], v, np.float32(-1e30))
            out[:, jj, :, :, d, :] = v.transpose(0, 3, 1, 2)
    return np.ascontiguousarray(out.reshape(L, 128, 4 * 14 * GW))


def _dup(a):
    return np.concatenate([a, a], axis=-2)


def _prep(inputs, SEQ, DEPTH):
    f = lambda a: np.ascontiguousarray(np.asarray(a, dtype=np.float32))
    L = DEPTH
    I = {kk: f(v) for kk, v in inputs.items()}
    C, S = _rope_tables(SEQ)
    common = {}
    for nm in ("w_ada", "w_in", "g_mla_q", "g_mla_kv", "g_diff", "w_branch", "w_out", "w_router", "e_bias", "w_e1", "w_e3", "w_e2",
               "w_s1", "w_s3", "w_s2", "g_final", "w_glu", "b_glu"):
        common[nm] = I[nm]
    common["w_uq"] = I["w_mla_uq"]
    common["w_ukv"] = I["w_mla_ukv"]
    common["b_adaT"] = f(I["b_ada"].reshape(L, 48, 128).transpose(0, 2, 1))
    common["g1T"] = f(I["g_norm1"].reshape(L, 8, 128).transpose(0, 2, 1))
    common["g2T"] = f(I["g_norm2"].reshape(L, 8, 128).transpose(0, 2, 1))
    common["ropeC"], common["ropeS"] = f(C), f(S)
    common["nab"] = _na_bias(I["na_rpb"])
    common["diff_lambda"] = f(I["diff_lambda"].reshape(L, 128))
    common["s5_d"] = f(I["s5_d"].reshape(L, 256))
    lr = I["s5_lam_re"].transpose(0, 1, 3, 2)
    li = I["s5_lam_im"].transpose(0, 1, 3, 2)
    stp = np.broadcast_to(I["s5_log_step"][:, :, None, :], lr.shape)
    common["s5p"] = f(np.concatenate([_dup(lr), _dup(li), _dup(stp)], axis=-1))
    br = I["s5_b_re"].transpose(0, 1, 3, 2, 4).reshape(L, 2, 64, 256)
    bi = I["s5_b_im"].transpose(0, 1, 3, 2, 4).reshape(L, 2, 64, 256)
    cr = I["s5_c_re"].transpose(0, 1, 4, 2, 3).reshape(L, 2, 64, 256)
    ci = I["s5_c_im"].transpose(0, 1, 4, 2, 3).reshape(L, 2, 64, 256)
    common["s5B"] = f(np.stack([np.concatenate([br, bi], 2), np.concatenate([bi, br], 2)], 2))
    common["s5C"] = f(np.stack([np.concatenate([cr, ci], 2), np.concatenate([ci, cr], 2)], 2))
    ar = np.arange(8, dtype=np.float32)
    kv = np.stack([7 - ar, ar + 1, ar - 7, ar, 8 - ar, -ar], 0).reshape(1, 48)
    common["s5kv"] = f(np.broadcast_to(kv, (128, 48)))
    s_i = np.arange(128) // 16
    common["s5msk"] = f(np.stack([(s_i[None, :] >= s_i[:, None]), (s_i[None, :] <= s_i[:, None])], 0).astype(np.float32))
    J = np.zeros((128, 128), np.float32)
    J[np.arange(64), np.arange(64) + 64] = 1
    J[np.arange(64) + 64, np.arange(64)] = 1
    common["s5J"] = J
    sg = np.ones((128, 2), np.float32)
    sg[:64, 0] = -1
    sg[64:, 1] = -1
    common["s5sg"] = sg
    maps = []
    for b in range(2):
        m = dict(common)
        m["x"] = f(I["x"][b])
        m["ctx"] = f(I["ctx"][b])
        cc = np.stack([I["c"][b], I["c_ctx"]], -1)
        m["cT"] = f(cc.reshape(8, 128, 2).transpose(1, 0, 2))
        maps.append(m)
    return maps


_CACHE = {}
_NOLAYER = ("x", "ctx", "cT", "ropeC", "ropeS", "g_final", "s5kv", "s5msk", "s5J", "s5sg", "lamc")


def kernel(**inputs):
    SEQ = inputs["x"].shape[1]
    DEPTH = inputs["w_in"].shape[0]
    if SEQ not in _CACHE:
        _CACHE[SEQ] = _build(SEQ, 1)
    kb = _CACHE[SEQ]
    maps = _prep(inputs, SEQ, DEPTH)
    xs = [m["x"] for m in maps]
    cs = [m["ctx"] for m in maps]
    ys = None
    for l in range(DEPTH):
        lam_init = 0.8 - 0.6 * math.exp(-0.3 * l)
        lamc = np.ascontiguousarray(np.broadcast_to(np.array([[-lam_init, 1.0 - lam_init]], np.float32), (64, 2)))
        lm = []
        for b in range(2):
            d = {}
            for n in kb.inp:
                if n == "x":
                    d[n] = xs[b]
                elif n == "ctx":
                    d[n] = cs[b]
                elif n == "lamc":
                    d[n] = lamc
                elif n in _NOLAYER:
                    d[n] = maps[b][n]
                else:
                    d[n] = np.ascontiguousarray(maps[b][n][l:l + 1])
            lm.append(d)
        res = run_bass_kernel_spmd(kb.nc, lm, core_ids=[0, 1])
        xs = [np.ascontiguousarray(np.asarray(res.results[b]["x_out"], dtype=np.float32)) for b in range(2)]
        cs = [np.ascontiguousarray(np.asarray(res.results[b]["ctx_out"], dtype=np.float32)) for b in range(2)]
        ys = [np.asarray(res.results[b]["y"], dtype=np.float32) for b in range(2)]
    return np.stack(ys, 0)
```
